# Optimizing a Trainium2 kernel written in Bass

```python
import jax, jax.numpy as jnp
from jax import lax
import numpy as np

D_MODEL = 1024
BATCH = 16
SEQ = 2048
DEPTH = 2

N_HEADS = 16
HEAD_DIM = D_MODEL // N_HEADS
D_FF = 2816
N_EXPERTS = 8
TOP_K = 2
D_FF_EXPERT = 2816
N_META = 16
BLOCK = 128
EPS = 1e-6
FORGET_BIAS_OFFSET = 3.0
N_A = DEPTH // 2
N_B = DEPTH - N_A
N_DENSE = (DEPTH + 1) // 2
N_MOE = DEPTH // 2

kernel_name = "yoco_stickbreak_fox_moe_trunk"


def rms_norm(x, g):
    xf = x.astype(jnp.float32)
    y = xf * lax.rsqrt(jnp.mean(xf * xf, axis=-1, keepdims=True) + EPS)
    return (y * g.astype(jnp.float32)).astype(x.dtype)


def swiglu(h, w_gate_up, w_down):
    gu = h @ w_gate_up
    g, u = jnp.split(gu, 2, axis=-1)
    return (jax.nn.silu(g) * u) @ w_down


def causal_block_sweep(block_fn, q_inputs, kv_inputs):
    B, L = q_inputs[0].shape[0], q_inputs[0].shape[1]
    pos = jnp.arange(L, dtype=jnp.int32)
    out_meta = block_fn([t[:, :N_META] for t in q_inputs], pos[:N_META],
                        [t[:, :N_META] for t in kv_inputs], pos[:N_META])
    n_blocks = (L - N_META) // BLOCK

    def to_blocks(t):
        r = t[:, N_META:]
        r = r.reshape((B, n_blocks, BLOCK) + r.shape[2:])
        return jnp.moveaxis(r, 1, 0)

    q_blocks = [to_blocks(t) for t in q_inputs]
    pos_blocks = pos[N_META:].reshape(n_blocks, BLOCK)
    out_blocks = lax.map(lambda a: block_fn(a[0], a[1], kv_inputs, pos), (q_blocks, pos_blocks))
    out_real = jnp.moveaxis(out_blocks, 0, 1).reshape((B, n_blocks * BLOCK) + out_blocks.shape[3:])
    return jnp.concatenate([out_meta, out_real], axis=1)


def stick_breaking_block(q_list, qpos, kv_list, kpos):
    (q,) = q_list
    k, v = kv_list
    z = jnp.einsum('bqhd,bkhd->bhqk', q.astype(jnp.float32), k.astype(jnp.float32)) * (HEAD_DIM ** -0.5)
    mask = kpos[None, :] < qpos[:, None]
    log1m = jnp.where(mask, -jax.nn.softplus(z), 0.0)
    after = lax.cumsum(log1m, axis=3, reverse=True) - log1m
    a = jnp.where(mask, jnp.exp(jax.nn.log_sigmoid(z) + after), 0.0)
    return jnp.einsum('bhqk,bkhd->bqhd', a, v.astype(jnp.float32)).astype(v.dtype)


def forgetting_block(q_list, qpos, kv_list, kpos):
    q, fq = q_list
    k, v, fk = kv_list
    logits = jnp.einsum('bqhd,bkhd->bhqk', q.astype(jnp.float32), k.astype(jnp.float32)) * (HEAD_DIM ** -0.5)
    decay = jnp.transpose(fq, (0, 2, 1))[:, :, :, None] - jnp.transpose(fk, (0, 2, 1))[:, :, None, :]
    mask = kpos[None, :] <= qpos[:, None]
    p = jax.nn.softmax(jnp.where(mask, logits + decay, -jnp.inf), axis=-1)
    return jnp.einsum('bhqk,bkhd->bqhd', p, v.astype(jnp.float32)).astype(v.dtype)


def stick_breaking_mixer(h, g, w_qkv, w_o):
    B, L, _ = h.shape
    qkv = (rms_norm(h, g) @ w_qkv).reshape(B, L, 3, N_HEADS, HEAD_DIM)
    q, k, v = qkv[:, :, 0], qkv[:, :, 1], qkv[:, :, 2]
    o = causal_block_sweep(stick_breaking_block, [q], [k, v])
    return o.reshape(B, L, D_MODEL) @ w_o


def shared_kv_state(h, g, w_kvf, b_f, k_norm):
    B, L, _ = h.shape
    u = rms_norm(h, g) @ w_kvf
    k = rms_norm(u[..., :D_MODEL].reshape(B, L, N_HEADS, HEAD_DIM), k_norm)
    v = u[..., D_MODEL:2 * D_MODEL].reshape(B, L, N_HEADS, HEAD_DIM)
    f_logit = (u[..., 2 * D_MODEL:] + b_f).astype(jnp.float32)
    F = lax.cumsum(jax.nn.log_sigmoid(f_logit), axis=1)
    return k, v, F


def forgetting_mixer(h, g, w_q, q_norm, w_o, k, v, F):
    B, L, _ = h.shape
    q = rms_norm((rms_norm(h, g) @ w_q).reshape(B, L, N_HEADS, HEAD_DIM), q_norm)
    o = causal_block_sweep(forgetting_block, [q, F], [k, v, F])
    return o.reshape(B, L, D_MODEL) @ w_o


def moe_swiglu(h, w_router, w_gu, w_down):
    B, L, D = h.shape
    t = h.reshape(B * L, D)
    logits = (t @ w_router).astype(jnp.float32)
    top_vals, top_idx = lax.top_k(logits, TOP_K)
    gates = jax.nn.softmax(top_vals, axis=-1)
    combine = jnp.sum(jax.nn.one_hot(top_idx, N_EXPERTS, dtype=jnp.float32) * gates[..., None], axis=1)
    y = jnp.zeros_like(t)
    for e in range(N_EXPERTS):
        y = y + combine[:, e:e + 1].astype(t.dtype) * swiglu(t, w_gu[e], w_down[e])
    return y.reshape(B, L, D)


def setup_inputs(seed: int = 0) -> dict:
    key = jax.random.key(seed)
    ks = jax.random.split(key, 24)
    D, H = D_MODEL, N_HEADS

    def w(k, shape, fan_in):
        return jax.random.normal(k, shape, jnp.float32) * (fan_in ** -0.5)

    def gain(k, shape):
        return 1.0 + 0.1 * jax.random.normal(k, shape, jnp.float32)

    return {
        "x": jax.random.normal(ks[0], (BATCH, SEQ, D), jnp.float32),
        "meta_tokens": jax.random.normal(ks[1], (N_META, D), jnp.float32),
        "norm_attn_a": gain(ks[2], (N_A, D)),
        "w_qkv_a": w(ks[3], (N_A, D, 3 * D), D),
        "w_o_a": w(ks[4], (N_A, D, D), D),
        "norm_kv": gain(ks[5], (D,)),
        "w_kvf": w(ks[6], (D, 2 * D + H), D),
        "b_f": FORGET_BIAS_OFFSET + 0.5 * jax.random.normal(ks[7], (H,), jnp.float32),
        "k_norm": gain(ks[8], (HEAD_DIM,)),
        "norm_attn_b": gain(ks[9], (N_B, D)),
        "w_q_b": w(ks[10], (N_B, D, D), D),
        "q_norm_b": gain(ks[11], (N_B, HEAD_DIM)),
        "w_o_b": w(ks[12], (N_B, D, D), D),
        "norm_ffn_dense": gain(ks[13], (N_DENSE, D)),
        "w_gu_dense": w(ks[14], (N_DENSE, D, 2 * D_FF), D),
        "w_down_dense": w(ks[15], (N_DENSE, D_FF, D), D_FF),
        "norm_ffn_moe": gain(ks[16], (N_MOE, D)),
        "w_router": w(ks[17], (N_MOE, D, N_EXPERTS), D),
        "w_gu_moe": w(ks[18], (N_MOE, N_EXPERTS, D, 2 * D_FF_EXPERT), D),
        "w_down_moe": w(ks[19], (N_MOE, N_EXPERTS, D_FF_EXPERT, D), D_FF_EXPERT),
    }


def reference(x, meta_tokens, norm_attn_a, w_qkv_a, w_o_a, norm_kv, w_kvf, b_f, k_norm,
              norm_attn_b, w_q_b, q_norm_b, w_o_b, norm_ffn_dense, w_gu_dense, w_down_dense,
              norm_ffn_moe, w_router, w_gu_moe, w_down_moe):
    B = x.shape[0]
    meta = jnp.broadcast_to(meta_tokens.astype(x.dtype)[None], (B, N_META, D_MODEL))
    h = jnp.concatenate([meta, x], axis=1)
    k_sh = v_sh = F_sh = None
    for layer in range(DEPTH):
        if layer < N_A:
            h = h + stick_breaking_mixer(h, norm_attn_a[layer], w_qkv_a[layer], w_o_a[layer])
        else:
            if layer == N_A:
                k_sh, v_sh, F_sh = shared_kv_state(h, norm_kv, w_kvf, b_f, k_norm)
            i = layer - N_A
            h = h + forgetting_mixer(h, norm_attn_b[i], w_q_b[i], q_norm_b[i], w_o_b[i], k_sh, v_sh, F_sh)
        j = layer // 2
        if layer % 2 == 0:
            h = h + swiglu(rms_norm(h, norm_ffn_dense[j]), w_gu_dense[j], w_down_dense[j])
        else:
            h = h + moe_swiglu(rms_norm(h, norm_ffn_moe[j]), w_router[j], w_gu_moe[j], w_down_moe[j])
    return h[:, N_META:]
```

```python
import bisect
from contextlib import ExitStack

import numpy as np
import concourse.bass as bass
import concourse.mybir as mybir
from concourse.bass_utils import run_bass_kernel_spmd

F32 = mybir.dt.float32
BF16 = mybir.dt.bfloat16
AF = mybir.ActivationFunctionType
ALU = mybir.AluOpType

D = 1024
NM = 16
SEQ = 2048
L = NM + SEQ
NH = 16
HD = 64
DFF = 2816
NE = 8
EPS = 1e-6
NCORES = 8

COLT = [(0, 16)] + [(16 + 512 * i, 512) for i in range(4)]
TOKT = [(0, 16)] + [(16 + 128 * i, 128) for i in range(16)]
UNITS = [(0, 4), (4, 4), (8, 4), (12, 4), (16, 3), (19, 3)]

C_ID, C_TRINEG, C_ONES, C_TRILE, C_BLK, C_NEG1, C_W = 0, 128, 256, 384, 512, 640, 768


def make_consts():
    c = np.zeros((128, C_W), np.float32)
    j = np.arange(128)[:, None]
    s = np.arange(128)[None, :]
    c[:, C_ID:C_ID + 128] = (j == s)
    c[:, C_TRINEG:C_TRINEG + 128] = -1.0 * (j >= s)
    c[:, C_ONES:C_ONES + 128] = 1.0
    c[:, C_TRILE:C_TRILE + 128] = 1.0 * (j <= s)
    c[:, C_BLK:C_BLK + 128] = 1.0 * ((j // 64) == (s // 64))
    c[:, C_NEG1:C_NEG1 + 128] = -1.0
    return c


class Res:
    __slots__ = ("w", "r", "gen")

    def __init__(self):
        self.w = None
        self.r = {}
        self.gen = -1


def mkres(n):
    return [Res() for _ in range(n)]


class Eng:
    def __init__(self, name, h):
        self.name = name
        self.h = h
        self.seq = 0
        self.sigs_seq = []
        self.sigs_ev = []
        self.sem = None
        self.cnt = 0
        self.waited = {}


class Sched:
    def __init__(self, nc, stack, n_dma_sems=48):
        self.nc = nc
        self.stack = stack
        self.sems = []
        self.pe = Eng("pe", nc.tensor)
        self.act = Eng("act", nc.scalar)
        self.dve = Eng("dve", nc.vector)
        self.pool = Eng("pool", nc.gpsimd)
        self.sp = Eng("sp", nc.sync)
        self.engs = [self.pe, self.act, self.dve, self.pool, self.sp]
        self.dmas_hw = [[self.new_sem("dqh%d" % i), 0] for i in range(n_dma_sems // 2)]
        self.dmas_sw = [[self.new_sem("dqs%d" % i), 0] for i in range(n_dma_sems // 2)]
        self.dmas = self.dmas_hw + self.dmas_sw
        self.rr_hw = 0
        self.rr_sw = 0
        self.gen = 0

    def new_sem(self, name):
        h = self.stack.enter_context(self.nc.semaphore(name))
        self.sems.append(h)
        return len(self.sems) - 1

    def _sig(self, E):
        if E.sem is None or E.cnt >= 30000:
            E.sem = self.new_sem("%s_e%d" % (E.name, len(self.sems)))
            E.cnt = 0
        E.cnt += 1
        return (E.sem, E.cnt)

    def _resolve(self, tok):
        if tok[0] == "d":
            return (tok[1], tok[2])
        E, seq = tok[1], tok[2]
        i = bisect.bisect_left(E.sigs_seq, seq)
        assert i < len(E.sigs_seq), "unresolved dep on %s seq %d" % (E.name, seq)
        return E.sigs_ev[i]

    def _wait(self, E, ev):
        s, v = ev
        if E.waited.get(s, 0) >= v:
            return
        E.h.wait_ge(self.sems[s], v)
        E.waited[s] = v

    def _deps(self, E, reads, writes):
        toks = []
        for r in reads:
            if r.gen == self.gen and r.w is not None:
                toks.append((r.w, 0))
        for w in writes:
            if w.gen == self.gen:
                if w.w is not None:
                    toks.append((w.w, 1))
                for t in w.r.values():
                    toks.append((t, 2))
        for tok, kind in toks:
            if tok[0] == "c" and tok[1] is E:
                if E is self.pe or kind == 2:
                    continue
            self._wait(E, self._resolve(tok))

    def _update(self, tok, reads, writes):
        key = tok[1] if tok[0] == "c" else ("d", tok[1])
        for r in reads:
            if r.gen != self.gen:
                r.gen = self.gen
                r.w = None
                r.r = {}
            r.r[key] = tok
        for w in writes:
            w.gen = self.gen
            w.w = tok
            w.r = {}

    def op(self, E, fn, reads=(), writes=(), signal=True):
        self._deps(E, reads, writes)
        ins = fn(E.h)
        E.seq += 1
        if signal:
            ev = self._sig(E)
            ins.then_inc(self.sems[ev[0]], 1)
            E.sigs_seq.append(E.seq)
            E.sigs_ev.append(ev)
        self._update(("c", E, E.seq), reads, writes)

    def dma(self, Q, out, in_, reads=(), writes=(), **kw):
        self._deps(Q, reads, writes)
        if Q is self.pool:
            slot = self.dmas_sw[self.rr_sw]
            self.rr_sw = (self.rr_sw + 1) % len(self.dmas_sw)
        else:
            slot = self.dmas_hw[self.rr_hw]
            self.rr_hw = (self.rr_hw + 1) % len(self.dmas_hw)
        self._wait(Q, (slot[0], slot[1]))
        slot[1] += 16
        Q.h.dma_start(out=out, in_=in_, **kw).then_inc(self.sems[slot[0]], 16)
        self._update(("d", slot[0], slot[1]), reads, writes)

    def barrier(self):
        evs = []
        for E in self.engs:
            if E.sigs_ev:
                assert E.sigs_seq[-1] == E.seq, "last instr of %s does not signal" % E.name
                evs.append(E.sigs_ev[-1])
        for s, v in self.dmas:
            if v:
                evs.append((s, v))
        for E in self.engs:
            for ev in evs:
                self._wait(E, ev)
        self.gen += 1


def pipeline(blocks, stages):
    n, k = len(blocks), len(stages)
    for step in range(n + k - 1):
        for si, st in enumerate(stages):
            i = step - si
            if 0 <= i < n:
                st(blocks[i])


def pipeline_rev(blocks, stages):
    n, k = len(blocks), len(stages)
    for step in range(n + k - 1):
        for si in range(k - 1, -1, -1):
            i = step - si
            if 0 <= i < n:
                stages[si](blocks[i])


class Rot:
    def __init__(self, items):
        self.items = items
        self.i = 0

    def next(self):
        it = self.items[self.i % len(self.items)]
        self.i += 1
        return it


def build_program(nseq=2, stop_after=99, debug=False):
    nc = bass.Bass("TRN2", target_bir_lowering=False)
    top = ExitStack()
    with top:
        _build(nc, top, nseq, stop_after, debug)
    return nc


def _build(nc, top, nseq, stop_after, debug):
    def din(name, shape):
        return nc.dram_tensor(name, list(shape), F32, kind="ExternalInput").ap()

    x_d = din("x", [nseq, SEQ, D])
    meta_d = din("meta_tokens", [NM, D])
    g_a_d = din("norm_attn_a", [1, D])
    wqkv_d = din("w_qkv_a", [1, D, 3 * D])
    woa_d = din("w_o_a", [1, D, D])
    g_kv_d = din("norm_kv", [1, D])
    wkvf_d = din("w_kvf", [D, 2 * D + NH])
    bf_d = din("b_f", [1, NH])
    kn_d = din("k_norm", [HD, 1])
    g_b_d = din("norm_attn_b", [1, D])
    wqb_d = din("w_q_b", [1, D, D])
    qn_d = din("q_norm_b", [HD, 1])
    wob_d = din("w_o_b", [1, D, D])
    g_fd_d = din("norm_ffn_dense", [1, D])
    wgud_d = din("w_gu_dense", [1, D, 2 * DFF])
    wdd_d = din("w_down_dense", [1, DFF, D])
    g_fm_d = din("norm_ffn_moe", [1, D])
    wr_d = din("w_router", [1, D, NE])
    wgum_d = din("w_gu_moe", [1, NE, D, 2 * DFF])
    wdm_d = din("w_down_moe", [1, NE, DFF, D])
    consts_d = din("consts", [128, C_W])
    out_d = nc.dram_tensor("out", [nseq, SEQ, D], F32, kind="ExternalOutput").ap()
    h2_d = nc.dram_tensor("h2s", [nseq, L, D], F32, kind="ExternalOutput" if debug else "Internal").ap()
    dbg = {}
    if debug:
        dbg["h1"] = nc.dram_tensor("dbg_h1", [nseq, L, D], F32, kind="ExternalOutput").ap()
        dbg["h3"] = nc.dram_tensor("dbg_h3", [nseq, SEQ, D], F32, kind="ExternalOutput").ap()
        dbg["comb"] = nc.dram_tensor("dbg_comb", [nseq, SEQ, NE], F32, kind="ExternalOutput").ap()

    S = Sched(nc, top)
    pe, act, dve, pool, sp = S.pe, S.act, S.dve, S.pool, S.sp

    uid = [0]

    def sb(stack, name, shape, dt):
        uid[0] += 1
        return stack.enter_context(nc.sbuf_tensor("%s_u%d" % (name, uid[0]), list(shape), dt))

    cf = sb(top, "cf", [128, C_W], F32)
    cb = sb(top, "cb", [128, C_W], BF16)
    r_c = Res()
    S.dma(sp, cf[:], consts_d[:, :], writes=[r_c])
    S.dma(pool, cb[:], consts_d[:, :], writes=[r_c])
    id_f = cf[:, C_ID:C_ID + 128]
    ones_f = cf[:, C_ONES:C_ONES + 128]
    trile_f = cf[:, C_TRILE:C_TRILE + 128]
    id_b = cb[:, C_ID:C_ID + 128]
    trineg_b = cb[:, C_TRINEG:C_TRINEG + 128]
    ones_b = cb[:, C_ONES:C_ONES + 128]
    blk_b = cb[:, C_BLK:C_BLK + 128]
    neg1_b = cb[:, C_NEG1:C_NEG1 + 128]

    PS = top.enter_context(nc.psum_tensor("ps_all", [128, 8, 512], F32))
    banks = [PS[:, i, :] for i in range(8)]
    r_bank = mkres(8)

    knorm2 = sb(top, "knorm2", [128, 1], F32)
    qnorm2 = sb(top, "qnorm2", [128, 1], F32)
    bfb = sb(top, "bfb", [128, NH], F32)
    wr32 = sb(top, "wr32", [128, 8, NE], F32)
    r_par = Res()
    for hh in range(2):
        S.dma(sp, knorm2[hh * 64:(hh + 1) * 64, :], kn_d[:, :], writes=[r_par])
        S.dma(sp, qnorm2[hh * 64:(hh + 1) * 64, :], qn_d[:, :], writes=[r_par])
    S.dma(sp, bfb[:], bf_d.partition_broadcast(128), writes=[r_par])
    S.dma(sp, wr32[:], wr_d[0].rearrange("(kc p) e -> p kc e", p=128), writes=[r_par])
    S.op(dve, lambda e: e.tensor_scalar(out=qnorm2[:], in0=qnorm2[:], scalar1=HD ** -0.5, scalar2=None, op0=ALU.mult),
         reads=[r_par], writes=[r_par])
    wrs = sb(top, "wrs", [128, 2, 8, NE], BF16)
    S.op(dve, lambda e: e.tensor_copy(out=wrs[:, 0], in_=wr32[:]), reads=[r_par], writes=[r_par])
    S.op(dve, lambda e: e.tensor_tensor(out=wrs[:, 1], in0=wr32[:], in1=wrs[:, 0], op=ALU.subtract),
         reads=[r_par], writes=[r_par])

    SelAll = sb(top, "SelAll", [NH, NH, 128], F32)
    S.op(dve, lambda e: e.tensor_copy(out=SelAll[:, :, :], in_=cf[0:NH, C_ID:C_ID + NH].unsqueeze(2).broadcast_to([NH, NH, 128])),
         reads=[r_c], writes=[r_par])

    def tiles_of(c0, n):
        return [t for t, (r0, r) in enumerate(TOKT) if r0 < c0 + n and r0 + r > c0]

    class NormWork:
        def __init__(self, stack, tag, nbuf=2, want32=False):
            self.n = nbuf
            self.junk = [sb(stack, "%s_junk%d" % (tag, i), [128, D], BF16) for i in range(nbuf)]
            self.ss = [sb(stack, "%s_ss%d" % (tag, i), [128, 4], F32) for i in range(nbuf)]
            self.xs = [sb(stack, "%s_xs%d" % (tag, i), [128, D], F32 if want32 else BF16) for i in range(nbuf)]
            self.res = [mkres(3) for _ in range(nbuf)]
            self.i = 0

    def norm_stats(nw, h_ap, r, r_h):
        k = nw.i % nw.n
        nw.i += 1
        rj, rs, rx = nw.res[k]
        ss = nw.ss[k]
        S.op(act, lambda e: e.activation(out=nw.junk[k][0:r, :], in_=h_ap, func=AF.Square, accum_out=ss[0:r, 0:1]),
             reads=[r_h], writes=[rj, rs])
        S.op(act, lambda e: e.activation(out=ss[0:r, 1:2], in_=ss[0:r, 0:1], func=AF.Ln, scale=1.0 / D, bias=EPS),
             reads=[rs], writes=[rs])
        S.op(act, lambda e: e.activation(out=ss[0:r, 2:3], in_=ss[0:r, 1:2], func=AF.Exp, scale=-0.5),
             reads=[rs], writes=[rs])
        return k, ss[0:r, 2:3], rs

    def norm_T_a(nw, k, rstd, r_rs, h_ap, r, r_h, gb, r_gb, bank_i):
        rx = nw.res[k][2]
        xs = nw.xs[k]
        S.op(dve, lambda e: e.scalar_tensor_tensor(out=xs[0:r, :], in0=h_ap, scalar=rstd, in1=gb[0:r, :],
                                                   op0=ALU.mult, op1=ALU.mult),
             reads=[r_h, r_rs, r_gb], writes=[rx])
        psT = banks[bank_i][:, :].bitcast(BF16).rearrange("p (c n) -> p c n", c=8)
        for c in range(8):
            S.op(pe, lambda e, c=c: e.transpose(out=psT[:, c, 0:r], in_=xs[0:r, c * 128:(c + 1) * 128],
                                                identity=id_b[0:r, 0:r]),
                 reads=[rx, r_c], writes=[r_bank[bank_i]], signal=(c == 7))

    def norm_T_b(r, xT, col0, r_dst, bank_i, eng=None):
        psT = banks[bank_i][:, :].bitcast(BF16).rearrange("p (c n) -> p c n", c=8)
        S.op(dve, lambda e: e.tensor_copy(out=xT[:, :, col0:col0 + r], in_=psT[:, :, 0:r]),
             reads=[r_bank[bank_i]], writes=[r_dst])

    def norm_T(nw, k, rstd, r_rs, h_ap, r, r_h, gb, r_gb, xT, col0, r_dst, bank_i):
        norm_T_a(nw, k, rstd, r_rs, h_ap, r, r_h, gb, r_gb, bank_i)
        norm_T_b(r, xT, col0, r_dst, bank_i)

    def norm_pipeline(nw, items):
        def s0(it):
            if it.get("pre"):
                it["pre"]()

        def s1(it):
            it["k"], it["rstd"], it["r_rs"] = norm_stats(nw, it["h"], it["r"], it["r_h"])

        def s2(it):
            for oi, (gb_, r_gb_, xT_, col0_, r_dst_, bank_) in enumerate(it["outs"]):
                k = it["k"]
                if oi > 0:
                    k = nw.i % nw.n
                    nw.i += 1
                norm_T_a(nw, k, it["rstd"], it["r_rs"], it["h"], it["r"], it["r_h"], gb_, r_gb_, bank_)

        def s3(it):
            for (gb_, r_gb_, xT_, col0_, r_dst_, bank_) in it["outs"]:
                norm_T_b(it["r"], xT_, col0_, r_dst_, bank_)
            if it.get("post"):
                it["post"]()

        pipeline_rev(items, [s0, s1, s2, s3])

    def load_gb(gb, r_gb, g_dram):
        S.dma(sp, gb[:], g_dram.partition_broadcast(128), writes=[r_gb])

    def h0_src(s, t):
        r0, r = TOKT[t]
        if t == 0:
            return meta_d[:, :]
        return x_d[s, r0 - NM:r0 - NM + r, :]

    def ffn_units(stack, xT, r_xT, coltiles, y, r_y, ytile_of, unit_srcs, scale_of, NW=3):
        wg = [sb(stack, "wg%d" % i, [128, 8, 512], BF16) for i in range(NW)]
        wu = [sb(stack, "wu%d" % i, [128, 8, 512], BF16) for i in range(NW)]
        wd = [sb(stack, "wd%d" % i, [128, 4, D], BF16) for i in range(NW)]
        r_w = mkres(NW)
        G = [sb(stack, "G%d" % i, [128, 4, 512], BF16) for i in range(2)]
        r_G = mkres(2)
        sg = Rot([(sb(stack, "sg%d" % i, [128, 512], BF16), Res()) for i in range(3)])
        gu_banks = Rot([0, 1, 2, 3])
        y_banks = Rot([4, 5, 6, 7])

        def load_unit(u):
            gd, ud, dd, nfc, _ = unit_srcs[u]
            k = u % NW
            fw = nfc * 128
            S.dma(pool, wg[k][:, :, 0:fw], gd.rearrange("(kc p) n -> p kc n", p=128), writes=[r_w[k]])
            S.dma(pool, wu[k][:, :, 0:fw], ud.rearrange("(kc p) n -> p kc n", p=128), writes=[r_w[k]])
            S.dma(pool, wd[k][:, 0:nfc, :], dd.rearrange("(fc p) n -> p fc n", p=128), writes=[r_w[k]])

        jobs = []
        for u in range(len(unit_srcs)):
            for ci, (c0, n) in enumerate(coltiles):
                jobs.append(dict(u=u, ci=ci, c0=c0, n=n, idx=len(jobs)))

        def stage_a(jb):
            u, c0, n = jb["u"], jb["c0"], jb["n"]
            if jb["ci"] == 0 and u == 0:
                load_unit(0)
                if len(unit_srcs) > 1:
                    load_unit(1)
            if jb["ci"] == 1 and u + 2 < len(unit_srcs):
                load_unit(u + 2)
            k = u % NW
            nfc = unit_srcs[u][3]
            gi = jb["idx"] % 2
            rx = [r_xT[t] for t in tiles_of(c0, n)]
            for fc in range(nfc):
                bg, bu = gu_banks.next(), gu_banks.next()
                for (bk, w) in ((bg, wg[k]), (bu, wu[k])):
                    for kc in range(8):
                        S.op(pe, lambda e, bk=bk, w=w, kc=kc: e.matmul(
                            banks[bk][:, 0:n], lhsT=w[:, kc, fc * 128:(fc + 1) * 128], rhs=xT[:, kc, c0:c0 + n],
                            start=(kc == 0), stop=(kc == 7)),
                             reads=[r_w[k]] + rx, writes=[r_bank[bk]], signal=(kc == 7))
                sgt, r_sg = sg.next()
                S.op(act, lambda e: e.activation(out=sgt[:, 0:n], in_=banks[bg][:, 0:n], func=AF.Silu),
                     reads=[r_bank[bg]], writes=[r_sg])
                S.op(dve, lambda e: e.tensor_tensor(out=G[gi][:, fc, 0:n], in0=banks[bu][:, 0:n], in1=sgt[:, 0:n],
                                                    op=ALU.mult),
                     reads=[r_bank[bu], r_sg], writes=[r_G[gi]])

        def stage_b(jb):
            u, c0, n = jb["u"], jb["c0"], jb["n"]
            k = u % NW
            nfc = unit_srcs[u][3]
            gi = jb["idx"] % 2
            for t in tiles_of(c0, n):
                r0, r = TOKT[t]
                off = r0 - c0
                yt = ytile_of(t)
                for half in range(2):
                    by = y_banks.next()
                    for fc in range(nfc):
                        S.op(pe, lambda e, fc=fc: e.matmul(
                            banks[by][0:r, :], lhsT=G[gi][:, fc, off:off + r], rhs=wd[k][:, fc, half * 512:(half + 1) * 512],
                            start=(fc == 0), stop=(fc == nfc - 1)),
                             reads=[r_G[gi], r_w[k]], writes=[r_bank[by]], signal=(fc == nfc - 1))
                    ysl = y[0:r, yt, half * 512:(half + 1) * 512]
                    sc = scale_of(unit_srcs[u][4], yt, r)
                    if sc is None:
                        S.op(dve, lambda e: e.tensor_tensor(out=ysl, in0=banks[by][0:r, :], in1=ysl, op=ALU.add),
                             reads=[r_bank[by], r_y[yt]], writes=[r_y[yt]])
                    else:
                        sc_ap, r_sc = sc
                        S.op(dve, lambda e: e.scalar_tensor_tensor(out=ysl, in0=banks[by][0:r, :], scalar=sc_ap, in1=ysl,
                                                                   op0=ALU.mult, op1=ALU.add),
                             reads=[r_bank[by], r_y[yt], r_sc], writes=[r_y[yt]])

        pipeline(jobs, [stage_a, stage_b])

    for s in range(nseq):
        with ExitStack() as l1:
            oT = sb(l1, "oT", [128, 8, L], BF16)
            r_oT = mkres(17)
            with ExitStack() as sa:
                xT = sb(sa, "xn0T", [128, 8, L], BF16)
                r_xT = mkres(17)
                gb = sb(sa, "gb0", [128, D], F32)
                r_gb = Res()
                load_gb(gb, r_gb, g_a_d)
                hbuf = [sb(sa, "hb%d" % i, [128, D], F32) for i in range(4)]
                r_hb = mkres(4)
                nw = NormWork(sa, "n0", nbuf=4)
                items = []
                for t, (r0, r) in enumerate(TOKT):
                    k3 = t % 4
                    items.append(dict(
                        pre=(lambda t=t, r=r, k3=k3: S.dma(sp, hbuf[k3][0:r, :], h0_src(s, t), writes=[r_hb[k3]])),
                        h=hbuf[k3][0:r, :], r=r, r_h=r_hb[k3],
                        outs=[(gb, r_gb, xT, r0, r_xT[t], 6 + (t % 2))]))
                norm_pipeline(nw, items)

                NB = 2
                wqkv = [sb(sa, "wqkv%d" % i, [128, 3, 8, 128], BF16) for i in range(NB)]
                r_wq = mkres(NB)
                QT = [sb(sa, "QT%d" % i, [128, L], BF16) for i in range(NB)]
                KT = [sb(sa, "KT%d" % i, [128, L], BF16) for i in range(NB)]
                V = [sb(sa, "V%d" % i, [128, 17, 128], BF16) for i in range(NB)]
                r_Q = [mkres(len(COLT)) for _ in range(NB)]
                r_K = [mkres(17) for _ in range(NB)]
                r_V = [mkres(17) for _ in range(NB)]
                Ebuf = Rot([(sb(sa, "E%d" % i, [128, 2, 512], F32), Res()) for i in range(5)])
                Pbuf = Rot([(sb(sa, "P%d" % i, [128, 2, 512], F32), Res()) for i in range(2)])
                SPbuf = Rot([(sb(sa, "SP%d" % i, [128, 2, 512], BF16), Res()) for i in range(2)])
                Abuf = Rot([(sb(sa, "A%d" % i, [128, 2, 512], BF16), Res()) for i in range(2)])
                Cbuf = [(sb(sa, "C%d" % i, [128, 2, 512], BF16), Res()) for i in range(2)]
                r_za, r_eb = Res(), Res()
                r_zas1 = mkres(2)
                NDUMMY_SB = 0
                wqv = wqkv_d[0].rearrange("(kc p) n -> p kc n", p=128)

                def load_pair_w(j):
                    jb = j % NB
                    for i in range(3):
                        S.dma(pool, wqkv[jb][:, i], wqv[:, :, i * D + j * 128:i * D + (j + 1) * 128], writes=[r_wq[jb]])

                load_pair_w(0)
                pbank = Rot([6, 7])
                for j in range(8):
                    jb = j % NB
                    if j + 1 < 8:
                        load_pair_w(j + 1)
                    for ci, (c0, n) in enumerate(COLT):
                        rx = [r_xT[t] for t in tiles_of(c0, n)]
                        for i, dst, rdst, scl in ((0, QT[jb], [r_Q[jb][ci]], HD ** -0.5),
                                                  (1, KT[jb], [r_K[jb][t] for t in tiles_of(c0, n)], 1.0)):
                            bk = pbank.next()
                            for kc in range(8):
                                S.op(pe, lambda e, kc=kc, i=i, bk=bk: e.matmul(
                                    banks[bk][:, 0:n], lhsT=wqkv[jb][:, i, kc, :], rhs=xT[:, kc, c0:c0 + n],
                                    start=(kc == 0), stop=(kc == 7)),
                                     reads=[r_wq[jb]] + rx, writes=[r_bank[bk]], signal=(kc == 7))
                            S.op(dve, lambda e, dst=dst, scl=scl, bk=bk: e.tensor_scalar(
                                out=dst[:, c0:c0 + n], in0=banks[bk][:, 0:n], scalar1=scl, scalar2=None, op0=ALU.mult),
                                 reads=[r_bank[bk]], writes=rdst)
                    for t, (r0, r) in enumerate(TOKT):
                        bk = pbank.next()
                        for kc in range(8):
                            S.op(pe, lambda e, kc=kc, bk=bk: e.matmul(
                                banks[bk][0:r, 0:128], lhsT=xT[:, kc, r0:r0 + r], rhs=wqkv[jb][:, 2, kc, :],
                                start=(kc == 0), stop=(kc == 7)),
                                 reads=[r_wq[jb], r_xT[t]], writes=[r_bank[bk]], signal=(kc == 7))
                        S.op(dve, lambda e, bk=bk: e.tensor_copy(out=V[jb][0:r, t, :], in_=banks[bk][0:r, 0:128]),
                             reads=[r_bank[bk]], writes=[r_V[jb][t]])

                    blocks = []
                    for qi, (qc0, n) in enumerate(COLT):
                        kts = [t for t in range(16, 0, -1) if TOKT[t][0] < qc0 + n] + [0]
                        if qi == 0:
                            kts = [0]
                        for bi, kt in enumerate(kts):
                            kc0, r = TOKT[kt]
                            qlo = max(0, kc0 - qc0)
                            masked = (kc0 + r - 1 >= qc0 + qlo)
                            blocks.append(dict(qi=qi, qc0=qc0, n=n, kt=kt, kc0=kc0, r=r, qlo=qlo, masked=masked,
                                               first=(bi == 0), last=(bi == len(kts) - 1)))
                    HP = (slice(0, 64), slice(64, 128))
                    for bi_, b_ in enumerate(blocks):
                        b_["zs"] = bi_ % 2

                    def st1(b):
                        r, qlo, n, kc0, qc0 = b["r"], b["qlo"], b["n"], b["kc0"], b["qc0"]
                        for hh in range(2):
                            S.op(pe, lambda e: e.matmul(PS[0:r, 2 * b["zs"] + hh, qlo:n], lhsT=KT[jb][HP[hh], kc0:kc0 + r],
                                                        rhs=QT[jb][HP[hh], qc0 + qlo:qc0 + n], start=True, stop=True),
                                 reads=[r_K[jb][b["kt"]], r_Q[jb][b["qi"]]], writes=[r_zas1[b["zs"]]], signal=(hh == 1))

                    def st2a(b):
                        r, qlo, n = b["r"], b["qlo"], b["n"]
                        Et, r_E = Ebuf.next()
                        b["E"], b["r_E"] = Et, r_E
                        zs = b["zs"]
                        S.op(act, lambda e: e.activation(out=Et[0:r, :, qlo:n], in_=PS[0:r, 2 * zs:2 * zs + 2, qlo:n], func=AF.Exp),
                             reads=[r_zas1[zs]], writes=[r_E])

                    def st2b(b):
                        r, qlo, n, kc0, qc0 = b["r"], b["qlo"], b["n"], b["kc0"], b["qc0"]
                        Et, r_E = b["E"], b["r_E"]
                        SPt, r_SP = SPbuf.next()
                        b["SP"], b["r_SP"] = SPt, r_SP
                        S.op(act, lambda e: e.activation(out=SPt[0:r, :, qlo:n], in_=Et[0:r, :, qlo:n], func=AF.Ln, bias=1.0),
                             reads=[r_E], writes=[r_SP])
                        if b["masked"]:
                            S.op(pool, lambda e: e.affine_select(out=SPt[0:r, :, qlo:n], in_=SPt[0:r, :, qlo:n],
                                                                 pattern=[[0, 2], [1, n - qlo]], compare_op=ALU.is_gt,
                                                                 fill=0.0, base=qc0 + qlo - kc0, channel_multiplier=-1),
                                 reads=[r_SP], writes=[r_SP])

                    def st3(b):
                        r, qlo, n, kc0, qc0 = b["r"], b["qlo"], b["n"], b["kc0"], b["qc0"]
                        Ct, r_C = Cbuf[b["qi"] % 2]
                        SPt, r_SP = b["SP"], b["r_SP"]
                        if b["first"] and not b["last"]:
                            S.op(dve, lambda e: e.memset(Ct[:, :, :], 0.0), writes=[r_C])
                        for hh in range(2):
                            S.op(pe, lambda e: e.matmul(PS[0:r, 4 + hh, qlo:n], lhsT=trineg_b[0:r, 0:r],
                                                        rhs=SPt[0:r, hh, qlo:n], start=True, stop=b["first"]),
                                 reads=[r_SP, r_c], writes=[r_eb], signal=(b["first"] and hh == 1))
                            if not b["first"]:
                                S.op(pe, lambda e: e.matmul(PS[0:r, 4 + hh, qlo:n], lhsT=neg1_b[:, 0:r], rhs=Ct[:, hh, qlo:n],
                                                            start=False, stop=True),
                                     reads=[r_C], writes=[r_eb], signal=(hh == 1))
                        if not b["last"]:
                            S.op(dve, lambda e: e.tensor_tensor(out=Ct[:, :, qlo:n], in0=Ct[:, :, qlo:n], in1=SPt[:, :, qlo:n],
                                                                op=ALU.add),
                                 reads=[r_C, r_SP], writes=[r_C])

                    def st4(b):
                        r, qlo, n, kc0, qc0 = b["r"], b["qlo"], b["n"], b["kc0"], b["qc0"]
                        At, r_A = Abuf.next()
                        b["A"], b["r_A"] = At, r_A
                        Pt, r_P = Pbuf.next()
                        Et, r_E = b["E"], b["r_E"]
                        S.op(act, lambda e: e.activation(out=Pt[0:r, :, qlo:n], in_=PS[0:r, 4:6, qlo:n], func=AF.Exp),
                             reads=[r_eb], writes=[r_P])
                        S.op(dve, lambda e: e.tensor_tensor(out=At[0:r, :, qlo:n], in0=Et[0:r, :, qlo:n], in1=Pt[0:r, :, qlo:n],
                                                            op=ALU.mult),
                             reads=[r_E, r_P], writes=[r_A])
                        if b["masked"]:
                            S.op(pool, lambda e: e.affine_select(out=At[0:r, :, qlo:n], in_=At[0:r, :, qlo:n],
                                                                 pattern=[[0, 2], [1, n - qlo]], compare_op=ALU.is_gt,
                                                                 fill=0.0, base=qc0 + qlo - kc0, channel_multiplier=-1),
                                 reads=[r_A], writes=[r_A])

                    def st5(b):
                        bo = 6 + (b["qi"] % 2)
                        r, qlo, n, qc0 = b["r"], b["qlo"], b["n"], b["qc0"]
                        At, r_A = b["A"], b["r_A"]
                        for hh in range(2):
                            S.op(pe, lambda e: e.matmul(PS[HP[hh], bo, qlo:n], lhsT=V[jb][0:r, b["kt"], HP[hh]],
                                                        rhs=At[0:r, hh, qlo:n], start=b["first"], stop=b["last"],
                                                        skip_group_check=True),
                                 reads=[r_V[jb][b["kt"]], r_A], writes=[r_bank[bo]], signal=(hh == 1))
                        if b["last"]:
                            S.op(dve, lambda e: e.tensor_copy(out=oT[:, j, qc0:qc0 + n], in_=PS[:, bo, 0:n]),
                                 reads=[r_bank[bo]], writes=[r_oT[t] for t in tiles_of(qc0, n)])

                    pipeline_rev(blocks, [st1, st2a, st2b, st3, st4, st5])
            S.barrier()
            if stop_after <= 1:
                continue

            with ExitStack() as sbk:
                y = sb(sbk, "y", [128, 17, D], F32)
                r_y = mkres(17)
                with ExitStack() as s2:
                    wo = sb(s2, "wo", [128, 8, D], BF16)
                    r_wo = Res()
                    S.dma(pool, wo[:], woa_d[0].rearrange("(kc p) n -> p kc n", p=128), writes=[r_wo])
                    gb = sb(s2, "gb1", [128, D], F32)
                    r_gb = Res()
                    load_gb(gb, r_gb, g_fd_d)
                    hbuf = [sb(s2, "hb%d" % i, [128, D], F32) for i in range(4)]
                    r_hb = mkres(4)
                    nw = NormWork(s2, "n1", nbuf=4)
                    obank = Rot([0, 1, 2, 3])
                    items = []
                    for t, (r0, r) in enumerate(TOKT):
                        k3 = t % 4

                        def pre(t=t, r0=r0, r=r, k3=k3):
                            S.dma(sp, hbuf[k3][0:r, :], h0_src(s, t), writes=[r_hb[k3]])
                            for half in range(2):
                                bk = obank.next()
                                for jj in range(8):
                                    S.op(pe, lambda e, jj=jj, bk=bk: e.matmul(
                                        banks[bk][0:r, :], lhsT=oT[:, jj, r0:r0 + r],
                                        rhs=wo[:, jj, half * 512:(half + 1) * 512], start=(jj == 0), stop=(jj == 7)),
                                         reads=[r_oT[t], r_wo], writes=[r_bank[bk]], signal=(jj == 7))
                                S.op(dve, lambda e, bk=bk: e.tensor_tensor(
                                    out=y[0:r, t, half * 512:(half + 1) * 512], in0=banks[bk][0:r, :],
                                    in1=hbuf[k3][0:r, half * 512:(half + 1) * 512], op=ALU.add),
                                     reads=[r_bank[bk], r_hb[k3]], writes=[r_y[t]])
                            if debug:
                                S.dma(sp, dbg["h1"][s, r0:r0 + r, :], y[0:r, t, :], reads=[r_y[t]])

                        items.append(dict(pre=pre, h=y[0:r, t, :], r=r, r_h=r_y[t],
                                          outs=[(gb, r_gb, oT, r0, r_oT[t], 6 + (t % 2))]))
                    norm_pipeline(nw, items)
                S.barrier()
                if stop_after >= 3:
                    with ExitStack() as s3:
                        srcs = []
                        for (f0, nfc) in UNITS:
                            srcs.append((wgud_d[0][:, f0 * 128:(f0 + nfc) * 128],
                                         wgud_d[0][:, DFF + f0 * 128:DFF + (f0 + nfc) * 128],
                                         wdd_d[0][f0 * 128:(f0 + nfc) * 128, :], nfc, None))
                        ffn_units(s3, oT, r_oT, COLT, y, r_y, lambda t: t, srcs, lambda key, yt, r: None)
                    for t, (r0, r) in enumerate(TOKT):
                        S.dma(sp, h2_d[s, r0:r0 + r, :], y[0:r, t, :], reads=[r_y[t]])
                S.barrier()
            if stop_after <= 3:
                continue

            with ExitStack() as sc:
                xkT = sb(sc, "xkvT", [128, 8, L], BF16)
                r_xk = mkres(17)
                xqT = sb(sc, "xqT", [128, 8, SEQ], BF16)
                r_xq = mkres(17)
                with ExitStack() as s4a:
                    gbk = sb(s4a, "gbk", [128, D], F32)
                    gbq = sb(s4a, "gbq", [128, D], F32)
                    r_gbk, r_gbq = Res(), Res()
                    load_gb(gbk, r_gbk, g_kv_d)
                    load_gb(gbq, r_gbq, g_b_d)
                    hbuf = [sb(s4a, "hb%d" % i, [128, D], F32) for i in range(4)]
                    r_hb = mkres(4)
                    nw = NormWork(s4a, "n4", nbuf=6)
                    items = []
                    for t, (r0, r) in enumerate(TOKT):
                        k3 = t % 4
                        outs = [(gbk, r_gbk, xkT, r0, r_xk[t], 6)]
                        if t > 0:
                            outs.append((gbq, r_gbq, xqT, r0 - NM, r_xq[t], 7))
                        items.append(dict(
                            pre=(lambda t=t, r0=r0, r=r, k3=k3: S.dma(sp, hbuf[k3][0:r, :], h2_d[s, r0:r0 + r, :],
                                                                        writes=[r_hb[k3]])),
                            h=hbuf[k3][0:r, :], r=r, r_h=r_hb[k3], outs=outs))
                    norm_pipeline(nw, items)
                S.barrier()

                wf = sb(sc, "wf", [128, 8, NH], BF16)
                r_wf = Res()
                S.dma(pool, wf[:], wkvf_d.rearrange("(kc p) n -> p kc n", p=128)[:, :, 2 * D:2 * D + NH], writes=[r_wf])
                spf = sb(sc, "spf", [128, 17, NH], F32)
                negF = sb(sc, "negF", [128, 17, NH], F32)
                Rb = sb(sc, "Rb", [128, 17, NH], F32)
                r_spf, r_negF, r_Rb = mkres(17), Res(), Res()
                fl = [sb(sc, "fl%d" % i, [128, 2, NH], F32) for i in range(2)]
                r_fl = mkres(2)
                S.op(dve, lambda e: e.memset(negF[:], 0.0), writes=[r_negF])
                for t, (r0, r) in enumerate(TOKT):
                    kf = t % 2
                    for kc in range(8):
                        S.op(pe, lambda e, kc=kc: e.matmul(banks[4][0:r, 0:NH], lhsT=xkT[:, kc, r0:r0 + r], rhs=wf[:, kc, :],
                                                           start=(kc == 0), stop=(kc == 7)),
                             reads=[r_xk[t], r_wf], writes=[r_bank[4]], signal=(kc == 7))
                    S.op(dve, lambda e: e.tensor_tensor(out=fl[kf][0:r, 0, :], in0=banks[4][0:r, 0:NH], in1=bfb[0:r, :],
                                                        op=ALU.add),
                         reads=[r_bank[4], r_par], writes=[r_fl[kf]])
                    S.op(act, lambda e: e.activation(out=fl[kf][0:r, 1, :], in_=fl[kf][0:r, 0, :], func=AF.Exp, scale=-1.0),
                         reads=[r_fl[kf]], writes=[r_fl[kf]])
                    S.op(act, lambda e: e.activation(out=spf[0:r, t, :], in_=fl[kf][0:r, 1, :], func=AF.Ln, bias=1.0),
                         reads=[r_fl[kf]], writes=[r_spf[t]])
                    S.op(pe, lambda e: e.matmul(banks[5][:, 0:NH], lhsT=ones_f[0:r, :], rhs=spf[0:r, t, :],
                                                start=True, stop=True),
                         reads=[r_spf[t], r_c], writes=[r_bank[5]])
                    S.op(pe, lambda e: e.matmul(banks[6][0:r, 0:NH], lhsT=trile_f[0:r, 0:r], rhs=spf[0:r, t, :],
                                                start=True, stop=True),
                         reads=[r_spf[t], r_c], writes=[r_bank[6]])
                    if t == 0:
                        S.op(dve, lambda e: e.tensor_copy(out=Rb[:, 0, :], in_=banks[5][:, 0:NH]),
                             reads=[r_bank[5]], writes=[r_Rb])
                        S.op(dve, lambda e: e.tensor_copy(out=negF[0:r, 0, :], in_=banks[6][0:r, 0:NH]),
                             reads=[r_bank[6], r_negF], writes=[r_negF])
                    else:
                        S.op(dve, lambda e: e.tensor_tensor(out=negF[0:r, t, :], in0=banks[6][0:r, 0:NH],
                                                            in1=Rb[0:r, t - 1, :], op=ALU.add),
                             reads=[r_bank[6], r_Rb, r_negF], writes=[r_negF])
                        S.op(dve, lambda e: e.tensor_tensor(out=Rb[:, t, :], in0=banks[5][:, 0:NH], in1=Rb[:, t - 1, :],
                                                            op=ALU.add),
                             reads=[r_bank[5], r_Rb], writes=[r_Rb])

                negFT = sb(sc, "negFT", [NH, L], F32)
                r_nFT = Res()
                for t, (r0, r) in enumerate(TOKT):
                    S.op(pe, lambda e: e.matmul(banks[7][0:NH, 0:r], lhsT=negF[0:r, t, :], rhs=id_f[0:r, 0:r],
                                                start=True, stop=True),
                         reads=[r_negF, r_c], writes=[r_bank[7]])
                    S.op(dve, lambda e: e.tensor_copy(out=negFT[:, r0:r0 + r], in_=banks[7][0:NH, 0:r]),
                         reads=[r_bank[7]], writes=[r_nFT])

                NB = 2
                wkv = [sb(sc, "wkv%d" % i, [128, 3, 8, 128], BF16) for i in range(NB)]
                r_wk = mkres(NB)
                QT = [sb(sc, "QT%d" % i, [128, SEQ], BF16) for i in range(NB)]
                KT = [sb(sc, "KT%d" % i, [128, L], BF16) for i in range(NB)]
                V = [sb(sc, "V%d" % i, [128, 17, 128], BF16) for i in range(NB)]
                r_Q = [mkres(4) for _ in range(NB)]
                r_K = [mkres(17) for _ in range(NB)]
                r_V = [mkres(17) for _ in range(NB)]
                sqb = Rot([(sb(sc, "sq%d" % i, [128, 512], BF16), Res()) for i in range(2)])
                rsb = Rot([(sb(sc, "rs%d" % i, [128, 2, 512], F32), Res()) for i in range(2)])
                ZZbuf = Rot([(sb(sc, "ZZ%d" % i, [128, 2, 512], F32), Res()) for i in range(2)])
                Abuf = Rot([(sb(sc, "A%d" % i, [128, 2, 512], BF16), Res()) for i in range(2)])
                FBbuf = [(sb(sc, "FB%d" % i, [128, 2, 512], F32), Res()) for i in range(2)]
                FKbuf = Rot([(sb(sc, "FK%d" % i, [128, 2, 512], F32), Res()) for i in range(3)])
                rden = Rot([(sb(sc, "rden%d" % i, [128, 512], F32), Res()) for i in range(2)])
                r_zas = mkres(2)
                NDUMMY_FOX = 1
                wkvv = wkvf_d.rearrange("(kc p) n -> p kc n", p=128)
                wqbv = wqb_d[0].rearrange("(kc p) n -> p kc n", p=128)

                def load_pair_w1(j):
                    jb = j % NB
                    S.dma(pool, wkv[jb][:, 0], wqbv[:, :, j * 128:(j + 1) * 128], writes=[r_wk[jb]])
                    S.dma(pool, wkv[jb][:, 1], wkvv[:, :, j * 128:(j + 1) * 128], writes=[r_wk[jb]])
                    S.dma(pool, wkv[jb][:, 2], wkvv[:, :, D + j * 128:D + (j + 1) * 128], writes=[r_wk[jb]])

                load_pair_w1(0)
                pbank = Rot([6, 7])
                sbank = Rot([4, 5])
                QCOL = [(512 * i, 512) for i in range(4)]
                for j in range(8):
                    jb = j % NB
                    if j + 1 < 8:
                        load_pair_w1(j + 1)

                    def proj_norm(i, src, r_src_of, c0, n, dst, rdst, nvec):
                        bk = pbank.next()
                        for kc in range(8):
                            S.op(pe, lambda e, kc=kc: e.matmul(banks[bk][:, 0:n], lhsT=wkv[jb][:, i, kc, :],
                                                               rhs=src[:, kc, c0:c0 + n], start=(kc == 0), stop=(kc == 7)),
                                 reads=[r_wk[jb]] + r_src_of, writes=[r_bank[bk]], signal=(kc == 7))
                        sqt, r_sq = sqb.next()
                        S.op(act, lambda e: e.activation(out=sqt[:, 0:n], in_=banks[bk][:, 0:n], func=AF.Square),
                             reads=[r_bank[bk]], writes=[r_sq])
                        bs = sbank.next()
                        S.op(pe, lambda e: e.matmul(banks[bs][:, 0:n], lhsT=blk_b, rhs=sqt[:, 0:n], start=True, stop=True),
                             reads=[r_sq, r_c], writes=[r_bank[bs]])
                        rst, r_rs = rsb.next()
                        S.op(act, lambda e: e.activation(out=rst[:, 0, 0:n], in_=banks[bs][:, 0:n], func=AF.Ln,
                                                         scale=1.0 / HD, bias=EPS),
                             reads=[r_bank[bs]], writes=[r_rs])
                        S.op(act, lambda e: e.activation(out=rst[:, 1, 0:n], in_=rst[:, 0, 0:n], func=AF.Exp, scale=-0.5),
                             reads=[r_rs], writes=[r_rs])
                        S.op(dve, lambda e: e.scalar_tensor_tensor(out=dst, in0=banks[bk][:, 0:n], scalar=nvec[:, 0:1],
                                                                   in1=rst[:, 1, 0:n], op0=ALU.mult, op1=ALU.mult),
                             reads=[r_bank[bk], r_rs, r_par], writes=rdst)

                    for ci, (c0, n) in enumerate(COLT):
                        tl = tiles_of(c0, n)
                        proj_norm(1, xkT, [r_xk[t] for t in tl], c0, n, KT[jb][:, c0:c0 + n], [r_K[jb][t] for t in tl], knorm2)
                    for ci, (c0, n) in enumerate(QCOL):
                        tl = [1 + 4 * ci + i for i in range(4)]
                        proj_norm(0, xqT, [r_xq[t] for t in tl], c0, n, QT[jb][:, c0:c0 + n], [r_Q[jb][ci]], qnorm2)
                    for t, (r0, r) in enumerate(TOKT):
                        bk = pbank.next()
                        for kc in range(8):
                            S.op(pe, lambda e, kc=kc, bk=bk: e.matmul(
                                banks[bk][0:r, 0:128], lhsT=xkT[:, kc, r0:r0 + r], rhs=wkv[jb][:, 2, kc, :],
                                start=(kc == 0), stop=(kc == 7)),
                                 reads=[r_wk[jb], r_xk[t]], writes=[r_bank[bk]], signal=(kc == 7))
                        S.op(dve, lambda e, bk=bk: e.tensor_copy(out=V[jb][0:r, t, :], in_=banks[bk][0:r, 0:128]),
                             reads=[r_bank[bk]], writes=[r_V[jb][t]])
                    blocks = []
                    for qi, (qc0, n) in enumerate(QCOL):
                        kts = [0] + [t for t in range(1, 17) if TOKT[t][0] - NM < qc0 + n]
                        for bi, kt in enumerate(kts):
                            kc0, r = TOKT[kt]
                            kq = kc0 - NM
                            qlo = max(0, kq - qc0)
                            masked = (kq + r - 1 >= qc0 + qlo)
                            blocks.append(dict(qi=qi, qc0=qc0, n=n, kt=kt, kc0=kc0, r=r, qlo=qlo, masked=masked,
                                               kq=kq, first=(bi == 0), last=(bi == len(kts) - 1)))
                    HP = (slice(0, 64), slice(64, 128))

                    def make_fb(qi):
                        qc0_, n_ = QCOL[qi]
                        FBt, r_FB = FBbuf[qi % 2]
                        for hh in range(2):
                            S.op(pe, lambda e: e.matmul(banks[6][:, :], lhsT=SelAll[:, 2 * j + hh, :],
                                                        rhs=negFT[:, NM + qc0_:NM + qc0_ + n_], start=True, stop=True),
                                 reads=[r_nFT, r_par], writes=[r_bank[6]])
                            S.op(dve, lambda e: e.tensor_scalar(out=FBt[:, hh, :], in0=banks[6][:, :], scalar1=-1.0,
                                                                scalar2=None, op0=ALU.mult),
                                 reads=[r_bank[6]], writes=[r_FB])

                    for bi_, b_ in enumerate(blocks):
                        b_["zs"] = bi_ % 2

                    def f1(b):
                        r, qlo, n, kc0, qc0 = b["r"], b["qlo"], b["n"], b["kc0"], b["qc0"]
                        zs = b["zs"]
                        for hh in range(2):
                            S.op(pe, lambda e: e.matmul(PS[0:r, 2 * zs + hh, qlo:n], lhsT=KT[jb][HP[hh], kc0:kc0 + r],
                                                        rhs=QT[jb][HP[hh], qc0 + qlo:qc0 + n], start=True, stop=True),
                                 reads=[r_K[jb][b["kt"]], r_Q[jb][b["qi"]]], writes=[r_zas[zs]], signal=(hh == 1))

                    def f0(b):
                        r, qlo, n = b["r"], b["qlo"], b["n"]
                        if b["first"]:
                            if b["qi"] == 0:
                                make_fb(0)
                            if b["qi"] + 1 < len(QCOL):
                                make_fb(b["qi"] + 1)
                        FBt, r_FB = FBbuf[b["qi"] % 2]
                        FKt, r_FK = FKbuf.next()
                        b["FK"], b["r_FK"] = FKt, r_FK
                        for hh in range(2):
                            h = 2 * j + hh
                            S.op(pool, lambda e: e.tensor_scalar(out=FKt[0:r, hh, qlo:n], in0=FBt[0:r, hh, qlo:n],
                                                                 scalar1=negF[0:r, b["kt"], h:h + 1], scalar2=1.0,
                                                                 op0=ALU.add, op1=ALU.mult),
                                 reads=[r_FB, r_negF], writes=[r_FK])

                    def f1b(b):
                        r, qlo, n = b["r"], b["qlo"], b["n"]
                        zs = b["zs"]
                        FKt, r_FK = b["FK"], b["r_FK"]
                        ZZt, r_ZZ = ZZbuf.next()
                        b["ZZ"], b["r_ZZ"] = ZZt, r_ZZ
                        S.op(dve, lambda e: e.tensor_tensor(out=ZZt[0:r, :, qlo:n], in0=PS[0:r, 2 * zs:2 * zs + 2, qlo:n],
                                                            in1=FKt[0:r, :, qlo:n], op=ALU.add),
                             reads=[r_zas[zs], r_FK], writes=[r_ZZ])

                    def f2(b):
                        r, qlo, n, qc0, kq = b["r"], b["qlo"], b["n"], b["qc0"], b["kq"]
                        ZZt, r_ZZ = b["ZZ"], b["r_ZZ"]
                        At, r_A = Abuf.next()
                        b["A"], b["r_A"] = At, r_A
                        S.op(act, lambda e: e.activation(out=At[0:r, :, qlo:n], in_=ZZt[0:r, :, qlo:n], func=AF.Exp),
                             reads=[r_ZZ], writes=[r_A])
                        if b["masked"]:
                            S.op(pool, lambda e: e.affine_select(out=At[0:r, :, qlo:qlo + 128], in_=At[0:r, :, qlo:qlo + 128],
                                                                 pattern=[[0, 2], [1, 128]], compare_op=ALU.is_ge, fill=0.0,
                                                                 base=qc0 + qlo - kq, channel_multiplier=-1),
                                 reads=[r_A], writes=[r_A])

                    def f3(b):
                        bn, bd = 4, 5
                        r, qlo, n, qc0 = b["r"], b["qlo"], b["n"], b["qc0"]
                        At, r_A = b["A"], b["r_A"]
                        for _ in range(NDUMMY_FOX):
                            S.op(pe, lambda e: e.matmul(banks[7][:, :], lhsT=cb[:, 0:128], rhs=KT[jb][:, 0:512],
                                                        start=True, stop=True),
                                 reads=[r_c] + [r_K[jb][t] for t in range(5)], writes=[r_bank[7]], signal=False)
                        for hh in range(2):
                            S.op(pe, lambda e: e.matmul(PS[HP[hh], bn, qlo:n], lhsT=V[jb][0:r, b["kt"], HP[hh]],
                                                        rhs=At[0:r, hh, qlo:n], start=b["first"], stop=b["last"],
                                                        skip_group_check=True),
                                 reads=[r_V[jb][b["kt"]], r_A], writes=[r_bank[bn]], signal=(hh == 1))
                        for hh in range(2):
                            S.op(pe, lambda e: e.matmul(PS[HP[hh], bd, qlo:n], lhsT=ones_b[0:r, 0:64],
                                                        rhs=At[0:r, hh, qlo:n], start=b["first"], stop=b["last"],
                                                        skip_group_check=True),
                                 reads=[r_A, r_c], writes=[r_bank[bd]], signal=(hh == 1))
                        if b["last"]:
                            rd, r_rd = rden.next()
                            S.op(dve, lambda e: e.reciprocal(out=rd[:, 0:n], in_=PS[:, bd, 0:n]),
                                 reads=[r_bank[bd]], writes=[r_rd])
                            S.op(dve, lambda e: e.tensor_tensor(out=oT[:, j, NM + qc0:NM + qc0 + n], in0=PS[:, bn, 0:n],
                                                                in1=rd[:, 0:n], op=ALU.mult),
                                 reads=[r_bank[bn], r_rd], writes=[r_oT[t] for t in tiles_of(NM + qc0, n)])

                    pipeline_rev(blocks, [f0, f1, f1b, f2, f3])
            S.barrier()
            if stop_after <= 4:
                continue

            with ExitStack() as sd:
                y = sb(sd, "y2", [128, 16, D], F32)
                r_y = mkres(16)
                comb = sb(sd, "comb", [128, 16, NE], F32)
                r_comb = mkres(16)
                with ExitStack() as s5:
                    wo = sb(s5, "wob", [128, 8, D], BF16)
                    r_wo = Res()
                    S.dma(pool, wo[:], wob_d[0].rearrange("(kc p) n -> p kc n", p=128), writes=[r_wo])
                    gb = sb(s5, "gbm", [128, D], F32)
                    r_gb = Res()
                    load_gb(gb, r_gb, g_fm_d)
                    hbuf = [sb(s5, "hb%d" % i, [128, D], F32) for i in range(4)]
                    r_hb = mkres(4)
                    nw = NormWork(s5, "n5", nbuf=3, want32=True)
                    xhl = [(sb(s5, "xh%d" % i, [128, D], BF16), sb(s5, "xl%d" % i, [128, D], BF16)) for i in range(2)]
                    r_xh, r_xl = mkres(2), mkres(2)
                    xlT = [sb(s5, "xlT%d" % i, [128, 8, 128], BF16) for i in range(2)]
                    r_xlT = mkres(2)
                    rt = [sb(s5, "rt%d" % i, [128, 5, NE], F32) for i in range(2)]
                    r_rt = mkres(2)
                    obank = Rot([0, 1, 2, 3])
                    items = [dict(t=t, r0=TOKT[t][0], yt=t - 1, k3=t % 4, k2=(t - 1) % 2) for t in range(1, 17)]

                    def p5_0(it):
                        t, r0, yt, k3 = it["t"], it["r0"], it["yt"], it["k3"]
                        S.dma(sp, hbuf[k3][:, :], h2_d[s, r0:r0 + 128, :], writes=[r_hb[k3]])
                        for half in range(2):
                            bk = obank.next()
                            for jj in range(8):
                                S.op(pe, lambda e, jj=jj, bk=bk: e.matmul(
                                    banks[bk][:, :], lhsT=oT[:, jj, r0:r0 + 128], rhs=wo[:, jj, half * 512:(half + 1) * 512],
                                    start=(jj == 0), stop=(jj == 7)),
                                     reads=[r_oT[t], r_wo], writes=[r_bank[bk]], signal=(jj == 7))
                            S.op(dve, lambda e, bk=bk: e.tensor_tensor(
                                out=y[:, yt, half * 512:(half + 1) * 512], in0=banks[bk][:, :],
                                in1=hbuf[k3][:, half * 512:(half + 1) * 512], op=ALU.add),
                                 reads=[r_bank[bk], r_hb[k3]], writes=[r_y[yt]])
                        if debug:
                            S.dma(sp, dbg["h3"][s, yt * 128:(yt + 1) * 128, :], y[:, yt, :], reads=[r_y[yt]])

                    def p5_1(it):
                        yt = it["yt"]
                        it["k"], it["rstd"], it["r_rs"] = norm_stats(nw, y[:, yt, :], 128, r_y[yt])

                    def p5_2(it):
                        yt, k2, k = it["yt"], it["k2"], it["k"]
                        rx = nw.res[k][2]
                        xs = nw.xs[k]
                        S.op(dve, lambda e: e.scalar_tensor_tensor(out=xs[:, :], in0=y[:, yt, :], scalar=it["rstd"], in1=gb[:, :],
                                                                   op0=ALU.mult, op1=ALU.mult),
                             reads=[r_y[yt], it["r_rs"], r_gb], writes=[rx])
                        xh, xl = xhl[k2]
                        S.op(act, lambda e: e.activation(out=xh[:, :], in_=xs[:, :], func=AF.Copy),
                             reads=[rx], writes=[r_xh[k2]])
                        S.op(dve, lambda e: e.tensor_tensor(out=xl[:, :], in0=xs[:, :], in1=xh[:, :], op=ALU.subtract),
                             reads=[rx, r_xh[k2]], writes=[r_xl[k2]])
                        for (srcx, r_srcx, bk) in ((xh, r_xh[k2], 4), (xl, r_xl[k2], 5)):
                            psT = banks[bk][:, :].bitcast(BF16).rearrange("p (c n) -> p c n", c=8)
                            for c in range(8):
                                S.op(pe, lambda e, c=c: e.transpose(out=psT[:, c, :], in_=srcx[:, c * 128:(c + 1) * 128],
                                                                    identity=id_b),
                                     reads=[r_srcx, r_c], writes=[r_bank[bk]], signal=(c == 7))

                    def p5_3(it):
                        t, r0, k2 = it["t"], it["r0"], it["k2"]
                        psT = banks[4][:, :].bitcast(BF16).rearrange("p (c n) -> p c n", c=8)
                        S.op(act, lambda e: e.activation(out=oT[:, :, r0:r0 + 128], in_=psT, func=AF.Copy),
                             reads=[r_bank[4]], writes=[r_oT[t]])
                        psT = banks[5][:, :].bitcast(BF16).rearrange("p (c n) -> p c n", c=8)
                        S.op(dve, lambda e: e.tensor_copy(out=xlT[k2][:, :, :], in_=psT),
                             reads=[r_bank[5]], writes=[r_xlT[k2]])
                        terms = [(oT, r_oT[t], r0, 0), (xlT[k2], r_xlT[k2], 0, 0), (oT, r_oT[t], r0, 1)]
                        for ti, (lt, r_lt, c0_, wi) in enumerate(terms):
                            for kc in range(8):
                                S.op(pe, lambda e, kc=kc: e.matmul(banks[6][:, 0:NE], lhsT=lt[:, kc, c0_:c0_ + 128],
                                                                   rhs=wrs[:, wi, kc, :],
                                                                   start=(ti == 0 and kc == 0), stop=(ti == 2 and kc == 7)),
                                     reads=[r_lt, r_par], writes=[r_bank[6]], signal=(ti == 2 and kc == 7))

                    def p5_4(it):
                        yt, k2 = it["yt"], it["k2"]
                        R_, rr_ = rt[k2], r_rt[k2]
                        S.op(dve, lambda e: e.tensor_copy(out=R_[:, 0, :], in_=banks[6][:, 0:NE]),
                             reads=[r_bank[6]], writes=[rr_])
                        S.op(dve, lambda e: e.max(out=R_[:, 1, :], in_=R_[:, 0, :]), reads=[rr_], writes=[rr_])
                        S.op(dve, lambda e: e.tensor_scalar(out=R_[:, 2, 0:1], in0=R_[:, 1, 0:1], scalar1=-1.0, scalar2=None,
                                                            op0=ALU.mult), reads=[rr_], writes=[rr_])
                        S.op(act, lambda e: e.activation(out=R_[:, 3, :], in_=R_[:, 0, :], func=AF.Exp, bias=R_[:, 2, 0:1]),
                             reads=[rr_], writes=[rr_])
                        S.op(dve, lambda e: e.scalar_tensor_tensor(out=R_[:, 4, :], in0=R_[:, 0, :], scalar=R_[:, 1, 1:2],
                                                                   in1=R_[:, 3, :], op0=ALU.is_ge, op1=ALU.mult,
                                                                   accum_out=R_[:, 2, 1:2]),
                             reads=[rr_], writes=[rr_])
                        S.op(dve, lambda e: e.reciprocal(out=R_[:, 2, 2:3], in_=R_[:, 2, 1:2]), reads=[rr_], writes=[rr_])
                        S.op(dve, lambda e: e.tensor_scalar(out=comb[:, yt, :], in0=R_[:, 4, :], scalar1=R_[:, 2, 2:3],
                                                            scalar2=None, op0=ALU.mult),
                             reads=[rr_], writes=[r_comb[yt]])
                        if debug:
                            S.dma(sp, dbg["comb"][s, yt * 128:(yt + 1) * 128, :], comb[:, yt, :], reads=[r_comb[yt]])

                    pipeline_rev(items, [p5_0, p5_1, p5_2, p5_3, p5_4])
                S.barrier()
                if stop_after >= 6:
                    with ExitStack() as s6:
                        srcs = []
                        for ex in range(NE):
                            for (f0, nfc) in UNITS:
                                srcs.append((wgum_d[0, ex][:, f0 * 128:(f0 + nfc) * 128],
                                             wgum_d[0, ex][:, DFF + f0 * 128:DFF + (f0 + nfc) * 128],
                                             wdm_d[0, ex][f0 * 128:(f0 + nfc) * 128, :], nfc, ex))
                        ffn_units(s6, oT, r_oT, COLT[1:], y, r_y, lambda t: t - 1, srcs,
                                  lambda key, yt, r: (comb[0:r, yt, key:key + 1], r_comb[yt]))
                for yt in range(16):
                    S.dma(sp, out_d[s, yt * 128:(yt + 1) * 128, :], y[:, yt, :], reads=[r_y[yt]])
                S.barrier()
    S.barrier()


_PROG = {}


def _in_map(inputs, b0, nseq):
    f = lambda a: np.ascontiguousarray(np.asarray(a, dtype=np.float32))
    return {
        "x": f(inputs["x"][b0:b0 + nseq]),
        "meta_tokens": f(inputs["meta_tokens"]),
        "norm_attn_a": f(inputs["norm_attn_a"]),
        "w_qkv_a": f(inputs["w_qkv_a"]),
        "w_o_a": f(inputs["w_o_a"]),
        "norm_kv": f(inputs["norm_kv"]).reshape(1, D),
        "w_kvf": f(inputs["w_kvf"]),
        "b_f": f(inputs["b_f"]).reshape(1, NH),
        "k_norm": f(inputs["k_norm"]).reshape(HD, 1),
        "norm_attn_b": f(inputs["norm_attn_b"]),
        "w_q_b": f(inputs["w_q_b"]),
        "q_norm_b": f(inputs["q_norm_b"]).reshape(HD, 1),
        "w_o_b": f(inputs["w_o_b"]),
        "norm_ffn_dense": f(inputs["norm_ffn_dense"]),
        "w_gu_dense": f(inputs["w_gu_dense"]),
        "w_down_dense": f(inputs["w_down_dense"]),
        "norm_ffn_moe": f(inputs["norm_ffn_moe"]),
        "w_router": f(inputs["w_router"]),
        "w_gu_moe": f(inputs["w_gu_moe"]),
        "w_down_moe": f(inputs["w_down_moe"]),
        "consts": make_consts(),
    }


def kernel(**inputs):
    nseq = 2
    nc = build_program(nseq=nseq)
    in_maps = [_in_map(inputs, nseq * c, nseq) for c in range(NCORES)]
    res = run_bass_kernel_spmd(nc, in_maps, core_ids=list(range(NCORES)))
    out = np.concatenate([np.asarray(r["out"]) for r in res.results], axis=0)
    return out.astype(np.float32)
```

```python
import bisect
from contextlib import ExitStack

import numpy as np
import concourse.bass as bass
import concourse.mybir as mybir
from concourse.bass_utils import run_bass_kernel_spmd

F32 = mybir.dt.float32
BF16 = mybir.dt.bfloat16
AF = mybir.ActivationFunctionType
ALU = mybir.AluOpType

D = 1024
NM = 16
SEQ = 2048
L = NM + SEQ
NH = 16
HD = 64
DFF = 2816
NE = 8
EPS = 1e-6
NCORES = 8

COLT = [(0, 16)] + [(16 + 512 * i, 512) for i in range(4)]
TOKT = [(0, 16)] + [(16 + 128 * i, 128) for i in range(16)]
UNITS = [(0, 4), (4, 4), (8, 4), (12, 4), (16, 3), (19, 3)]

C_ID, C_TRINEG, C_ONES, C_TRILE, C_BLK, C_NEG1, C_W = 0, 128, 256, 384, 512, 640, 768


def make_consts():
    c = np.zeros((128, C_W), np.float32)
    j = np.arange(128)[:, None]
    s = np.arange(128)[None, :]
    c[:, C_ID:C_ID + 128] = (j == s)
    c[:, C_TRINEG:C_TRINEG + 128] = -1.0 * (j >= s)
    c[:, C_ONES:C_ONES + 128] = 1.0
    c[:, C_TRILE:C_TRILE + 128] = 1.0 * (j <= s)
    c[:, C_BLK:C_BLK + 128] = 1.0 * ((j // 64) == (s // 64))
    c[:, C_NEG1:C_NEG1 + 128] = -1.0
    return c


class Res:
    __slots__ = ("w", "r", "gen")

    def __init__(self):
        self.w = None
        self.r = {}
        self.gen = -1


def mkres(n):
    return [Res() for _ in range(n)]


class Eng:
    def __init__(self, name, h):
        self.name = name
        self.h = h
        self.seq = 0
        self.sigs_seq = []
        self.sigs_ev = []
        self.sem = None
        self.cnt = 0
        self.waited = {}


class Sched:
    def __init__(self, nc, stack, n_dma_sems=48):
        self.nc = nc
        self.stack = stack
        self.sems = []
        self.pe = Eng("pe", nc.tensor)
        self.act = Eng("act", nc.scalar)
        self.dve = Eng("dve", nc.vector)
        self.pool = Eng("pool", nc.gpsimd)
        self.sp = Eng("sp", nc.sync)
        self.engs = [self.pe, self.act, self.dve, self.pool, self.sp]
        self.dmas_hw = [[self.new_sem("dqh%d" % i), 0] for i in range(n_dma_sems // 2)]
        self.dmas_sw = [[self.new_sem("dqs%d" % i), 0] for i in range(n_dma_sems // 2)]
        self.dmas = self.dmas_hw + self.dmas_sw
        self.rr_hw = 0
        self.rr_sw = 0
        self.gen = 0

    def new_sem(self, name):
        h = self.stack.enter_context(self.nc.semaphore(name))
        self.sems.append(h)
        return len(self.sems) - 1

    def _sig(self, E):
        if E.sem is None or E.cnt >= 30000:
            E.sem = self.new_sem("%s_e%d" % (E.name, len(self.sems)))
            E.cnt = 0
        E.cnt += 1
        return (E.sem, E.cnt)

    def _resolve(self, tok):
        if tok[0] == "d":
            return (tok[1], tok[2])
        E, seq = tok[1], tok[2]
        i = bisect.bisect_left(E.sigs_seq, seq)
        assert i < len(E.sigs_seq), "unresolved dep on %s seq %d" % (E.name, seq)
        return E.sigs_ev[i]

    def _wait(self, E, ev):
        s, v = ev
        if E.waited.get(s, 0) >= v:
            return
        E.h.wait_ge(self.sems[s], v)
        E.waited[s] = v

    def _deps(self, E, reads, writes):
        toks = []
        for r in reads:
            if r.gen == self.gen and r.w is not None:
                toks.append((r.w, 0))
        for w in writes:
            if w.gen == self.gen:
                if w.w is not None:
                    toks.append((w.w, 1))
                for t in w.r.values():
                    toks.append((t, 2))
        for tok, kind in toks:
            if tok[0] == "c" and tok[1] is E:
                if E is self.pe or kind == 2:
                    continue
            self._wait(E, self._resolve(tok))

    def _update(self, tok, reads, writes):
        key = tok[1] if tok[0] == "c" else ("d", tok[1])
        for r in reads:
            if r.gen != self.gen:
                r.gen = self.gen
                r.w = None
                r.r = {}
            r.r[key] = tok
        for w in writes:
            w.gen = self.gen
            w.w = tok
            w.r = {}

    def op(self, E, fn, reads=(), writes=(), signal=True):
        self._deps(E, reads, writes)
        ins = fn(E.h)
        E.seq += 1
        if signal:
            ev = self._sig(E)
            ins.then_inc(self.sems[ev[0]], 1)
            E.sigs_seq.append(E.seq)
            E.sigs_ev.append(ev)
        self._update(("c", E, E.seq), reads, writes)

    def dma(self, Q, out, in_, reads=(), writes=(), **kw):
        self._deps(Q, reads, writes)
        if Q is self.pool:
            slot = self.dmas_sw[self.rr_sw]
            self.rr_sw = (self.rr_sw + 1) % len(self.dmas_sw)
        else:
            slot = self.dmas_hw[self.rr_hw]
            self.rr_hw = (self.rr_hw + 1) % len(self.dmas_hw)
        self._wait(Q, (slot[0], slot[1]))
        slot[1] += 16
        Q.h.dma_start(out=out, in_=in_, **kw).then_inc(self.sems[slot[0]], 16)
        self._update(("d", slot[0], slot[1]), reads, writes)

    def barrier(self):
        evs = []
        for E in self.engs:
            if E.sigs_ev:
                assert E.sigs_seq[-1] == E.seq, "last instr of %s does not signal" % E.name
                evs.append(E.sigs_ev[-1])
        for s, v in self.dmas:
            if v:
                evs.append((s, v))
        for E in self.engs:
            for ev in evs:
                self._wait(E, ev)
        self.gen += 1


def pipeline(blocks, stages):
    n, k = len(blocks), len(stages)
    for step in range(n + k - 1):
        for si, st in enumerate(stages):
            i = step - si
            if 0 <= i < n:
                st(blocks[i])


def pipeline_rev(blocks, stages):
    n, k = len(blocks), len(stages)
    for step in range(n + k - 1):
        for si in range(k - 1, -1, -1):
            i = step - si
            if 0 <= i < n:
                stages[si](blocks[i])


class Rot:
    def __init__(self, items):
        self.items = items
        self.i = 0

    def next(self):
        it = self.items[self.i % len(self.items)]
        self.i += 1
        return it


def build_program(nseq=2, stop_after=99, debug=False):
    nc = bass.Bass("TRN2", target_bir_lowering=False)
    top = ExitStack()
    with top:
        _build(nc, top, nseq, stop_after, debug)
    return nc


def _build(nc, top, nseq, stop_after, debug):
    def din(name, shape):
        return nc.dram_tensor(name, list(shape), F32, kind="ExternalInput").ap()

    x_d = din("x", [nseq, SEQ, D])
    meta_d = din("meta_tokens", [NM, D])
    g_a_d = din("norm_attn_a", [1, D])
    wqkv_d = din("w_qkv_a", [1, D, 3 * D])
    woa_d = din("w_o_a", [1, D, D])
    g_kv_d = din("norm_kv", [1, D])
    wkvf_d = din("w_kvf", [D, 2 * D + NH])
    bf_d = din("b_f", [1, NH])
    kn_d = din("k_norm", [HD, 1])
    g_b_d = din("norm_attn_b", [1, D])
    wqb_d = din("w_q_b", [1, D, D])
    qn_d = din("q_norm_b", [HD, 1])
    wob_d = din("w_o_b", [1, D, D])
    g_fd_d = din("norm_ffn_dense", [1, D])
    wgud_d = din("w_gu_dense", [1, D, 2 * DFF])
    wdd_d = din("w_down_dense", [1, DFF, D])
    g_fm_d = din("norm_ffn_moe", [1, D])
    wr_d = din("w_router", [1, D, NE])
    wgum_d = din("w_gu_moe", [1, NE, D, 2 * DFF])
    wdm_d = din("w_down_moe", [1, NE, DFF, D])
    consts_d = din("consts", [128, C_W])
    out_d = nc.dram_tensor("out", [nseq, SEQ, D], F32, kind="ExternalOutput").ap()
    h2_d = nc.dram_tensor("h2s", [nseq, L, D], F32, kind="ExternalOutput" if debug else "Internal").ap()
    dbg = {}
    if debug:
        dbg["h1"] = nc.dram_tensor("dbg_h1", [nseq, L, D], F32, kind="ExternalOutput").ap()
        dbg["h3"] = nc.dram_tensor("dbg_h3", [nseq, SEQ, D], F32, kind="ExternalOutput").ap()
        dbg["comb"] = nc.dram_tensor("dbg_comb", [nseq, SEQ, NE], F32, kind="ExternalOutput").ap()

    S = Sched(nc, top)
    pe, act, dve, pool, sp = S.pe, S.act, S.dve, S.pool, S.sp

    uid = [0]

    def sb(stack, name, shape, dt):
        uid[0] += 1
        return stack.enter_context(nc.sbuf_tensor("%s_u%d" % (name, uid[0]), list(shape), dt))

    cf = sb(top, "cf", [128, C_W], F32)
    cb = sb(top, "cb", [128, C_W], BF16)
    r_c = Res()
    S.dma(sp, cf[:], consts_d[:, :], writes=[r_c])
    S.dma(pool, cb[:], consts_d[:, :], writes=[r_c])
    id_f = cf[:, C_ID:C_ID + 128]
    ones_f = cf[:, C_ONES:C_ONES + 128]
    trile_f = cf[:, C_TRILE:C_TRILE + 128]
    id_b = cb[:, C_ID:C_ID + 128]
    trineg_b = cb[:, C_TRINEG:C_TRINEG + 128]
    ones_b = cb[:, C_ONES:C_ONES + 128]
    blk_b = cb[:, C_BLK:C_BLK + 128]
    neg1_b = cb[:, C_NEG1:C_NEG1 + 128]

    PS = top.enter_context(nc.psum_tensor("ps_all", [128, 8, 512], F32))
    banks = [PS[:, i, :] for i in range(8)]
    r_bank = mkres(8)

    knorm2 = sb(top, "knorm2", [128, 1], F32)
    qnorm2 = sb(top, "qnorm2", [128, 1], F32)
    bfb = sb(top, "bfb", [128, NH], F32)
    wr32 = sb(top, "wr32", [128, 8, NE], F32)
    r_par = Res()
    for hh in range(2):
        S.dma(sp, knorm2[hh * 64:(hh + 1) * 64, :], kn_d[:, :], writes=[r_par])
        S.dma(sp, qnorm2[hh * 64:(hh + 1) * 64, :], qn_d[:, :], writes=[r_par])
    S.dma(sp, bfb[:], bf_d.partition_broadcast(128), writes=[r_par])
    S.dma(sp, wr32[:], wr_d[0].rearrange("(kc p) e -> p kc e", p=128), writes=[r_par])
    S.op(dve, lambda e: e.tensor_scalar(out=qnorm2[:], in0=qnorm2[:], scalar1=HD ** -0.5, scalar2=None, op0=ALU.mult),
         reads=[r_par], writes=[r_par])
    wrs = sb(top, "wrs", [128, 2, 8, NE], BF16)
    S.op(dve, lambda e: e.tensor_copy(out=wrs[:, 0], in_=wr32[:]), reads=[r_par], writes=[r_par])
    S.op(dve, lambda e: e.tensor_tensor(out=wrs[:, 1], in0=wr32[:], in1=wrs[:, 0], op=ALU.subtract),
         reads=[r_par], writes=[r_par])

    SelAll = sb(top, "SelAll", [NH, NH, 128], F32)
    S.op(dve, lambda e: e.tensor_copy(out=SelAll[:, :, :], in_=cf[0:NH, C_ID:C_ID + NH].unsqueeze(2).broadcast_to([NH, NH, 128])),
         reads=[r_c], writes=[r_par])

    def tiles_of(c0, n):
        return [t for t, (r0, r) in enumerate(TOKT) if r0 < c0 + n and r0 + r > c0]

    class NormWork:
        def __init__(self, stack, tag, nbuf=2, want32=False):
            self.n = nbuf
            self.junk = [sb(stack, "%s_junk%d" % (tag, i), [128, D], BF16) for i in range(nbuf)]
            self.ss = [sb(stack, "%s_ss%d" % (tag, i), [128, 4], F32) for i in range(nbuf)]
            self.xs = [sb(stack, "%s_xs%d" % (tag, i), [128, D], F32 if want32 else BF16) for i in range(nbuf)]
            self.res = [mkres(3) for _ in range(nbuf)]
            self.i = 0

    def norm_stats(nw, h_ap, r, r_h):
        k = nw.i % nw.n
        nw.i += 1
        rj, rs, rx = nw.res[k]
        ss = nw.ss[k]
        S.op(act, lambda e: e.activation(out=nw.junk[k][0:r, :], in_=h_ap, func=AF.Square, accum_out=ss[0:r, 0:1]),
             reads=[r_h], writes=[rj, rs])
        S.op(act, lambda e: e.activation(out=ss[0:r, 1:2], in_=ss[0:r, 0:1], func=AF.Ln, scale=1.0 / D, bias=EPS),
             reads=[rs], writes=[rs])
        S.op(act, lambda e: e.activation(out=ss[0:r, 2:3], in_=ss[0:r, 1:2], func=AF.Exp, scale=-0.5),
             reads=[rs], writes=[rs])
        return k, ss[0:r, 2:3], rs

    def norm_T_a(nw, k, rstd, r_rs, h_ap, r, r_h, gb, r_gb, bank_i):
        rx = nw.res[k][2]
        xs = nw.xs[k]
        S.op(dve, lambda e: e.scalar_tensor_tensor(out=xs[0:r, :], in0=h_ap, scalar=rstd, in1=gb[0:r, :],
                                                   op0=ALU.mult, op1=ALU.mult),
             reads=[r_h, r_rs, r_gb], writes=[rx])
        psT = banks[bank_i][:, :].bitcast(BF16).rearrange("p (c n) -> p c n", c=8)
        for c in range(8):
            S.op(pe, lambda e, c=c: e.transpose(out=psT[:, c, 0:r], in_=xs[0:r, c * 128:(c + 1) * 128],
                                                identity=id_b[0:r, 0:r]),
                 reads=[rx, r_c], writes=[r_bank[bank_i]], signal=(c == 7))

    def norm_T_b(r, xT, col0, r_dst, bank_i, eng=None):
        psT = banks[bank_i][:, :].bitcast(BF16).rearrange("p (c n) -> p c n", c=8)
        S.op(dve, lambda e: e.tensor_copy(out=xT[:, :, col0:col0 + r], in_=psT[:, :, 0:r]),
             reads=[r_bank[bank_i]], writes=[r_dst])

    def norm_T(nw, k, rstd, r_rs, h_ap, r, r_h, gb, r_gb, xT, col0, r_dst, bank_i):
        norm_T_a(nw, k, rstd, r_rs, h_ap, r, r_h, gb, r_gb, bank_i)
        norm_T_b(r, xT, col0, r_dst, bank_i)

    def norm_pipeline(nw, items):
        def s0(it):
            if it.get("pre"):
                it["pre"]()

        def s1(it):
            it["k"], it["rstd"], it["r_rs"] = norm_stats(nw, it["h"], it["r"], it["r_h"])

        def s2(it):
            for oi, (gb_, r_gb_, xT_, col0_, r_dst_, bank_) in enumerate(it["outs"]):
                k = it["k"]
                if oi > 0:
                    k = nw.i % nw.n
                    nw.i += 1
                norm_T_a(nw, k, it["rstd"], it["r_rs"], it["h"], it["r"], it["r_h"], gb_, r_gb_, bank_)

        def s3(it):
            for (gb_, r_gb_, xT_, col0_, r_dst_, bank_) in it["outs"]:
                norm_T_b(it["r"], xT_, col0_, r_dst_, bank_)
            if it.get("post"):
                it["post"]()

        pipeline_rev(items, [s0, s1, s2, s3])

    def load_gb(gb, r_gb, g_dram):
        S.dma(sp, gb[:], g_dram.partition_broadcast(128), writes=[r_gb])

    def h0_src(s, t):
        r0, r = TOKT[t]
        if t == 0:
            return meta_d[:, :]
        return x_d[s, r0 - NM:r0 - NM + r, :]

    def ffn_units(stack, xT, r_xT, coltiles, y, r_y, ytile_of, unit_srcs, scale_of, NW=3):
        wg = [sb(stack, "wg%d" % i, [128, 8, 512], BF16) for i in range(NW)]
        wu = [sb(stack, "wu%d" % i, [128, 8, 512], BF16) for i in range(NW)]
        wd = [sb(stack, "wd%d" % i, [128, 4, D], BF16) for i in range(NW)]
        r_w = mkres(NW)
        G = [sb(stack, "G%d" % i, [128, 4, 512], BF16) for i in range(2)]
        r_G = mkres(2)
        sg = Rot([(sb(stack, "sg%d" % i, [128, 512], BF16), Res()) for i in range(3)])
        gu_banks = Rot([0, 1, 2, 3])
        y_banks = Rot([4, 5, 6, 7])

        def load_unit(u):
            gd, ud, dd, nfc, _ = unit_srcs[u]
            k = u % NW
            fw = nfc * 128
            S.dma(pool, wg[k][:, :, 0:fw], gd.rearrange("(kc p) n -> p kc n", p=128), writes=[r_w[k]])
            S.dma(pool, wu[k][:, :, 0:fw], ud.rearrange("(kc p) n -> p kc n", p=128), writes=[r_w[k]])
            S.dma(pool, wd[k][:, 0:nfc, :], dd.rearrange("(fc p) n -> p fc n", p=128), writes=[r_w[k]])

        jobs = []
        for u in range(len(unit_srcs)):
            for ci, (c0, n) in enumerate(coltiles):
                jobs.append(dict(u=u, ci=ci, c0=c0, n=n, idx=len(jobs)))

        def stage_a(jb):
            u, c0, n = jb["u"], jb["c0"], jb["n"]
            if jb["ci"] == 0 and u == 0:
                load_unit(0)
                if len(unit_srcs) > 1:
                    load_unit(1)
            if jb["ci"] == 1 and u + 2 < len(unit_srcs):
                load_unit(u + 2)
            k = u % NW
            nfc = unit_srcs[u][3]
            gi = jb["idx"] % 2
            rx = [r_xT[t] for t in tiles_of(c0, n)]
            for fc in range(nfc):
                bg, bu = gu_banks.next(), gu_banks.next()
                for (bk, w) in ((bg, wg[k]), (bu, wu[k])):
                    for kc in range(8):
                        S.op(pe, lambda e, bk=bk, w=w, kc=kc: e.matmul(
                            banks[bk][:, 0:n], lhsT=w[:, kc, fc * 128:(fc + 1) * 128], rhs=xT[:, kc, c0:c0 + n],
                            start=(kc == 0), stop=(kc == 7)),
                             reads=[r_w[k]] + rx, writes=[r_bank[bk]], signal=(kc == 7))
                sgt, r_sg = sg.next()
                S.op(act, lambda e: e.activation(out=sgt[:, 0:n], in_=banks[bg][:, 0:n], func=AF.Silu),
                     reads=[r_bank[bg]], writes=[r_sg])
                S.op(dve, lambda e: e.tensor_tensor(out=G[gi][:, fc, 0:n], in0=banks[bu][:, 0:n], in1=sgt[:, 0:n],
                                                    op=ALU.mult),
                     reads=[r_bank[bu], r_sg], writes=[r_G[gi]])

        def stage_b(jb):
            u, c0, n = jb["u"], jb["c0"], jb["n"]
            k = u % NW
            nfc = unit_srcs[u][3]
            gi = jb["idx"] % 2
            for t in tiles_of(c0, n):
                r0, r = TOKT[t]
                off = r0 - c0
                yt = ytile_of(t)
                for half in range(2):
                    by = y_banks.next()
                    for fc in range(nfc):
                        S.op(pe, lambda e, fc=fc: e.matmul(
                            banks[by][0:r, :], lhsT=G[gi][:, fc, off:off + r], rhs=wd[k][:, fc, half * 512:(half + 1) * 512],
                            start=(fc == 0), stop=(fc == nfc - 1)),
                             reads=[r_G[gi], r_w[k]], writes=[r_bank[by]], signal=(fc == nfc - 1))
                    ysl = y[0:r, yt, half * 512:(half + 1) * 512]
                    sc = scale_of(unit_srcs[u][4], yt, r)
                    if sc is None:
                        S.op(dve, lambda e: e.tensor_tensor(out=ysl, in0=banks[by][0:r, :], in1=ysl, op=ALU.add),
                             reads=[r_bank[by], r_y[yt]], writes=[r_y[yt]])
                    else:
                        sc_ap, r_sc = sc
                        S.op(dve, lambda e: e.scalar_tensor_tensor(out=ysl, in0=banks[by][0:r, :], scalar=sc_ap, in1=ysl,
                                                                   op0=ALU.mult, op1=ALU.add),
                             reads=[r_bank[by], r_y[yt], r_sc], writes=[r_y[yt]])

        pipeline(jobs, [stage_a, stage_b])

    for s in range(nseq):
        with ExitStack() as l1:
            oT = sb(l1, "oT", [128, 8, L], BF16)
            r_oT = mkres(17)
            with ExitStack() as sa:
                xT = sb(sa, "xn0T", [128, 8, L], BF16)
                r_xT = mkres(17)
                gb = sb(sa, "gb0", [128, D], F32)
                r_gb = Res()
                load_gb(gb, r_gb, g_a_d)
                hbuf = [sb(sa, "hb%d" % i, [128, D], F32) for i in range(4)]
                r_hb = mkres(4)
                nw = NormWork(sa, "n0", nbuf=4)
                items = []
                for t, (r0, r) in enumerate(TOKT):
                    k3 = t % 4
                    items.append(dict(
                        pre=(lambda t=t, r=r, k3=k3: S.dma(sp, hbuf[k3][0:r, :], h0_src(s, t), writes=[r_hb[k3]])),
                        h=hbuf[k3][0:r, :], r=r, r_h=r_hb[k3],
                        outs=[(gb, r_gb, xT, r0, r_xT[t], 6 + (t % 2))]))
                norm_pipeline(nw, items)

                NB = 2
                wqkv = [sb(sa, "wqkv%d" % i, [128, 3, 8, 128], BF16) for i in range(NB)]
                r_wq = mkres(NB)
                QT = [sb(sa, "QT%d" % i, [128, L], BF16) for i in range(NB)]
                KT = [sb(sa, "KT%d" % i, [128, L], BF16) for i in range(NB)]
                V = [sb(sa, "V%d" % i, [128, 17, 128], BF16) for i in range(NB)]
                r_Q = [mkres(len(COLT)) for _ in range(NB)]
                r_K = [mkres(17) for _ in range(NB)]
                r_V = [mkres(17) for _ in range(NB)]
                Ebuf = Rot([(sb(sa, "E%d" % i, [128, 2, 512], F32), Res()) for i in range(5)])
                Pbuf = Rot([(sb(sa, "P%d" % i, [128, 2, 512], F32), Res()) for i in range(2)])
                SPbuf = Rot([(sb(sa, "SP%d" % i, [128, 2, 512], BF16), Res()) for i in range(2)])
                Abuf = Rot([(sb(sa, "A%d" % i, [128, 2, 512], BF16), Res()) for i in range(2)])
                Cbuf = [(sb(sa, "C%d" % i, [128, 2, 512], BF16), Res()) for i in range(2)]
                r_za, r_eb = Res(), Res()
                r_zas1 = mkres(2)
                NDUMMY_SB = 0
                wqv = wqkv_d[0].rearrange("(kc p) n -> p kc n", p=128)

                def load_pair_w(j):
                    jb = j % NB
                    for i in range(3):
                        S.dma(pool, wqkv[jb][:, i], wqv[:, :, i * D + j * 128:i * D + (j + 1) * 128], writes=[r_wq[jb]])

                load_pair_w(0)
                pbank = Rot([6, 7])
                for j in range(8):
                    jb = j % NB
                    if j + 1 < 8:
                        load_pair_w(j + 1)
                    for ci, (c0, n) in enumerate(COLT):
                        rx = [r_xT[t] for t in tiles_of(c0, n)]
                        for i, dst, rdst, scl in ((0, QT[jb], [r_Q[jb][ci]], HD ** -0.5),
                                                  (1, KT[jb], [r_K[jb][t] for t in tiles_of(c0, n)], 1.0)):
                            bk = pbank.next()
                            for kc in range(8):
                                S.op(pe, lambda e, kc=kc, i=i, bk=bk: e.matmul(
                                    banks[bk][:, 0:n], lhsT=wqkv[jb][:, i, kc, :], rhs=xT[:, kc, c0:c0 + n],
                                    start=(kc == 0), stop=(kc == 7)),
                                     reads=[r_wq[jb]] + rx, writes=[r_bank[bk]], signal=(kc == 7))
                            S.op(dve, lambda e, dst=dst, scl=scl, bk=bk: e.tensor_scalar(
                                out=dst[:, c0:c0 + n], in0=banks[bk][:, 0:n], scalar1=scl, scalar2=None, op0=ALU.mult),
                                 reads=[r_bank[bk]], writes=rdst)
                    for t, (r0, r) in enumerate(TOKT):
                        bk = pbank.next()
                        for kc in range(8):
                            S.op(pe, lambda e, kc=kc, bk=bk: e.matmul(
                                banks[bk][0:r, 0:128], lhsT=xT[:, kc, r0:r0 + r], rhs=wqkv[jb][:, 2, kc, :],
                                start=(kc == 0), stop=(kc == 7)),
                                 reads=[r_wq[jb], r_xT[t]], writes=[r_bank[bk]], signal=(kc == 7))
                        S.op(dve, lambda e, bk=bk: e.tensor_copy(out=V[jb][0:r, t, :], in_=banks[bk][0:r, 0:128]),
                             reads=[r_bank[bk]], writes=[r_V[jb][t]])

                    blocks = []
                    for qi, (qc0, n) in enumerate(COLT):
                        kts = [t for t in range(16, 0, -1) if TOKT[t][0] < qc0 + n] + [0]
                        if qi == 0:
                            kts = [0]
                        for bi, kt in enumerate(kts):
                            kc0, r = TOKT[kt]
                            qlo = max(0, kc0 - qc0)
                            masked = (kc0 + r - 1 >= qc0 + qlo)
                            blocks.append(dict(qi=qi, qc0=qc0, n=n, kt=kt, kc0=kc0, r=r, qlo=qlo, masked=masked,
                                               first=(bi == 0), last=(bi == len(kts) - 1)))
                    HP = (slice(0, 64), slice(64, 128))
                    for bi_, b_ in enumerate(blocks):
                        b_["zs"] = bi_ % 2

                    def st1(b):
                        r, qlo, n, kc0, qc0 = b["r"], b["qlo"], b["n"], b["kc0"], b["qc0"]
                        for hh in range(2):
                            S.op(pe, lambda e: e.matmul(PS[0:r, 2 * b["zs"] + hh, qlo:n], lhsT=KT[jb][HP[hh], kc0:kc0 + r],
                                                        rhs=QT[jb][HP[hh], qc0 + qlo:qc0 + n], start=True, stop=True),
                                 reads=[r_K[jb][b["kt"]], r_Q[jb][b["qi"]]], writes=[r_zas1[b["zs"]]], signal=(hh == 1))

                    def st2a(b):
                        r, qlo, n = b["r"], b["qlo"], b["n"]
                        Et, r_E = Ebuf.next()
                        b["E"], b["r_E"] = Et, r_E
                        zs = b["zs"]
                        S.op(act, lambda e: e.activation(out=Et[0:r, :, qlo:n], in_=PS[0:r, 2 * zs:2 * zs + 2, qlo:n], func=AF.Exp),
                             reads=[r_zas1[zs]], writes=[r_E])

                    def st2b(b):
                        r, qlo, n, kc0, qc0 = b["r"], b["qlo"], b["n"], b["kc0"], b["qc0"]
                        Et, r_E = b["E"], b["r_E"]
                        SPt, r_SP = SPbuf.next()
                        b["SP"], b["r_SP"] = SPt, r_SP
                        S.op(act, lambda e: e.activation(out=SPt[0:r, :, qlo:n], in_=Et[0:r, :, qlo:n], func=AF.Ln, bias=1.0),
                             reads=[r_E], writes=[r_SP])
                        if b["masked"]:
                            S.op(pool, lambda e: e.affine_select(out=SPt[0:r, :, qlo:n], in_=SPt[0:r, :, qlo:n],
                                                                 pattern=[[0, 2], [1, n - qlo]], compare_op=ALU.is_gt,
                                                                 fill=0.0, base=qc0 + qlo - kc0, channel_multiplier=-1),
                                 reads=[r_SP], writes=[r_SP])

                    def st3(b):
                        r, qlo, n, kc0, qc0 = b["r"], b["qlo"], b["n"], b["kc0"], b["qc0"]
                        Ct, r_C = Cbuf[b["qi"] % 2]
                        SPt, r_SP = b["SP"], b["r_SP"]
                        if b["first"] and not b["last"]:
                            S.op(dve, lambda e: e.memset(Ct[:, :, :], 0.0), writes=[r_C])
                        for hh in range(2):
                            S.op(pe, lambda e: e.matmul(PS[0:r, 4 + hh, qlo:n], lhsT=trineg_b[0:r, 0:r],
                                                        rhs=SPt[0:r, hh, qlo:n], start=True, stop=b["first"]),
                                 reads=[r_SP, r_c], writes=[r_eb], signal=(b["first"] and hh == 1))
                            if not b["first"]:
                                S.op(pe, lambda e: e.matmul(PS[0:r, 4 + hh, qlo:n], lhsT=neg1_b[:, 0:r], rhs=Ct[:, hh, qlo:n],
                                                            start=False, stop=True),
                                     reads=[r_C], writes=[r_eb], signal=(hh == 1))
                        if not b["last"]:
                            S.op(dve, lambda e: e.tensor_tensor(out=Ct[:, :, qlo:n], in0=Ct[:, :, qlo:n], in1=SPt[:, :, qlo:n],
                                                                op=ALU.add),
                                 reads=[r_C, r_SP], writes=[r_C])

                    def st4(b):
                        r, qlo, n, kc0, qc0 = b["r"], b["qlo"], b["n"], b["kc0"], b["qc0"]
                        At, r_A = Abuf.next()
                        b["A"], b["r_A"] = At, r_A
                        Pt, r_P = Pbuf.next()
                        Et, r_E = b["E"], b["r_E"]
                        S.op(act, lambda e: e.activation(out=Pt[0:r, :, qlo:n], in_=PS[0:r, 4:6, qlo:n], func=AF.Exp),
                             reads=[r_eb], writes=[r_P])
                        S.op(dve, lambda e: e.tensor_tensor(out=At[0:r, :, qlo:n], in0=Et[0:r, :, qlo:n], in1=Pt[0:r, :, qlo:n],
                                                            op=ALU.mult),
                             reads=[r_E, r_P], writes=[r_A])
                        if b["masked"]:
                            S.op(pool, lambda e: e.affine_select(out=At[0:r, :, qlo:n], in_=At[0:r, :, qlo:n],
                                                                 pattern=[[0, 2], [1, n - qlo]], compare_op=ALU.is_gt,
                                                                 fill=0.0, base=qc0 + qlo - kc0, channel_multiplier=-1),
                                 reads=[r_A], writes=[r_A])

                    def st5(b):
                        bo = 6 + (b["qi"] % 2)
                        r, qlo, n, qc0 = b["r"], b["qlo"], b["n"], b["qc0"]
                        At, r_A = b["A"], b["r_A"]
                        for hh in range(2):
                            S.op(pe, lambda e: e.matmul(PS[HP[hh], bo, qlo:n], lhsT=V[jb][0:r, b["kt"], HP[hh]],
                                                        rhs=At[0:r, hh, qlo:n], start=b["first"], stop=b["last"],
                                                        skip_group_check=True),
                                 reads=[r_V[jb][b["kt"]], r_A], writes=[r_bank[bo]], signal=(hh == 1))
                        if b["last"]:
                            S.op(dve, lambda e: e.tensor_copy(out=oT[:, j, qc0:qc0 + n], in_=PS[:, bo, 0:n]),
                                 reads=[r_bank[bo]], writes=[r_oT[t] for t in tiles_of(qc0, n)])

                    pipeline_rev(blocks, [st1, st2a, st2b, st3, st4, st5])
            S.barrier()
            if stop_after <= 1:
                continue

            with ExitStack() as sbk:
                y = sb(sbk, "y", [128, 17, D], F32)
                r_y = mkres(17)
                with ExitStack() as s2:
                    wo = sb(s2, "wo", [128, 8, D], BF16)
                    r_wo = Res()
                    S.dma(pool, wo[:], woa_d[0].rearrange("(kc p) n -> p kc n", p=128), writes=[r_wo])
                    gb = sb(s2, "gb1", [128, D], F32)
                    r_gb = Res()
                    load_gb(gb, r_gb, g_fd_d)
                    hbuf = [sb(s2, "hb%d" % i, [128, D], F32) for i in range(4)]
                    r_hb = mkres(4)
                    nw = NormWork(s2, "n1", nbuf=4)
                    obank = Rot([0, 1, 2, 3])
                    items = []
                    for t, (r0, r) in enumerate(TOKT):
                        k3 = t % 4

                        def pre(t=t, r0=r0, r=r, k3=k3):
                            S.dma(sp, hbuf[k3][0:r, :], h0_src(s, t), writes=[r_hb[k3]])
                            for half in range(2):
                                bk = obank.next()
                                for jj in range(8):
                                    S.op(pe, lambda e, jj=jj, bk=bk: e.matmul(
                                        banks[bk][0:r, :], lhsT=oT[:, jj, r0:r0 + r],
                                        rhs=wo[:, jj, half * 512:(half + 1) * 512], start=(jj == 0), stop=(jj == 7)),
                                         reads=[r_oT[t], r_wo], writes=[r_bank[bk]], signal=(jj == 7))
                                S.op(dve, lambda e, bk=bk: e.tensor_tensor(
                                    out=y[0:r, t, half * 512:(half + 1) * 512], in0=banks[bk][0:r, :],
                                    in1=hbuf[k3][0:r, half * 512:(half + 1) * 512], op=ALU.add),
                                     reads=[r_bank[bk], r_hb[k3]], writes=[r_y[t]])
                            if debug:
                                S.dma(sp, dbg["h1"][s, r0:r0 + r, :], y[0:r, t, :], reads=[r_y[t]])

                        items.append(dict(pre=pre, h=y[0:r, t, :], r=r, r_h=r_y[t],
                                          outs=[(gb, r_gb, oT, r0, r_oT[t], 6 + (t % 2))]))
                    norm_pipeline(nw, items)
                S.barrier()
                if stop_after >= 3:
                    with ExitStack() as s3:
                        srcs = []
                        for (f0, nfc) in UNITS:
                            srcs.append((wgud_d[0][:, f0 * 128:(f0 + nfc) * 128],
                                         wgud_d[0][:, DFF + f0 * 128:DFF + (f0 + nfc) * 128],
                                         wdd_d[0][f0 * 128:(f0 + nfc) * 128, :], nfc, None))
                        ffn_units(s3, oT, r_oT, COLT, y, r_y, lambda t: t, srcs, lambda key, yt, r: None)
                    for t, (r0, r) in enumerate(TOKT):
                        S.dma(sp, h2_d[s, r0:r0 + r, :], y[0:r, t, :], reads=[r_y[t]])
                S.barrier()
            if stop_after <= 3:
                continue

            with ExitStack() as sc:
                xkT = sb(sc, "xkvT", [128, 8, L], BF16)
                r_xk = mkres(17)
                xqT = sb(sc, "xqT", [128, 8, SEQ], BF16)
                r_xq = mkres(17)
                with ExitStack() as s4a:
                    gbk = sb(s4a, "gbk", [128, D], F32)
                    gbq = sb(s4a, "gbq", [128, D], F32)
                    r_gbk, r_gbq = Res(), Res()
                    load_gb(gbk, r_gbk, g_kv_d)
                    load_gb(gbq, r_gbq, g_b_d)
                    hbuf = [sb(s4a, "hb%d" % i, [128, D], F32) for i in range(4)]
                    r_hb = mkres(4)
                    nw = NormWork(s4a, "n4", nbuf=6)
                    items = []
                    for t, (r0, r) in enumerate(TOKT):
                        k3 = t % 4
                        outs = [(gbk, r_gbk, xkT, r0, r_xk[t], 6)]
                        if t > 0:
                            outs.append((gbq, r_gbq, xqT, r0 - NM, r_xq[t], 7))
                        items.append(dict(
                            pre=(lambda t=t, r0=r0, r=r, k3=k3: S.dma(sp, hbuf[k3][0:r, :], h2_d[s, r0:r0 + r, :],
                                                                        writes=[r_hb[k3]])),
                            h=hbuf[k3][0:r, :], r=r, r_h=r_hb[k3], outs=outs))
                    norm_pipeline(nw, items)
                S.barrier()

                wf = sb(sc, "wf", [128, 8, NH], BF16)
                r_wf = Res()
                S.dma(pool, wf[:], wkvf_d.rearrange("(kc p) n -> p kc n", p=128)[:, :, 2 * D:2 * D + NH], writes=[r_wf])
                spf = sb(sc, "spf", [128, 17, NH], F32)
                negF = sb(sc, "negF", [128, 17, NH], F32)
                Rb = sb(sc, "Rb", [128, 17, NH], F32)
                r_spf, r_negF, r_Rb = mkres(17), Res(), Res()
                fl = [sb(sc, "fl%d" % i, [128, 2, NH], F32) for i in range(2)]
                r_fl = mkres(2)
                S.op(dve, lambda e: e.memset(negF[:], 0.0), writes=[r_negF])
                for t, (r0, r) in enumerate(TOKT):
                    kf = t % 2
                    for kc in range(8):
                        S.op(pe, lambda e, kc=kc: e.matmul(banks[4][0:r, 0:NH], lhsT=xkT[:, kc, r0:r0 + r], rhs=wf[:, kc, :],
                                                           start=(kc == 0), stop=(kc == 7)),
                             reads=[r_xk[t], r_wf], writes=[r_bank[4]], signal=(kc == 7))
                    S.op(dve, lambda e: e.tensor_tensor(out=fl[kf][0:r, 0, :], in0=banks[4][0:r, 0:NH], in1=bfb[0:r, :],
                                                        op=ALU.add),
                         reads=[r_bank[4], r_par], writes=[r_fl[kf]])
                    S.op(act, lambda e: e.activation(out=fl[kf][0:r, 1, :], in_=fl[kf][0:r, 0, :], func=AF.Exp, scale=-1.0),
                         reads=[r_fl[kf]], writes=[r_fl[kf]])
                    S.op(act, lambda e: e.activation(out=spf[0:r, t, :], in_=fl[kf][0:r, 1, :], func=AF.Ln, bias=1.0),
                         reads=[r_fl[kf]], writes=[r_spf[t]])
                    S.op(pe, lambda e: e.matmul(banks[5][:, 0:NH], lhsT=ones_f[0:r, :], rhs=spf[0:r, t, :],
                                                start=True, stop=True),
                         reads=[r_spf[t], r_c], writes=[r_bank[5]])
                    S.op(pe, lambda e: e.matmul(banks[6][0:r, 0:NH], lhsT=trile_f[0:r, 0:r], rhs=spf[0:r, t, :],
                                                start=True, stop=True),
                         reads=[r_spf[t], r_c], writes=[r_bank[6]])
                    if t == 0:
                        S.op(dve, lambda e: e.tensor_copy(out=Rb[:, 0, :], in_=banks[5][:, 0:NH]),
                             reads=[r_bank[5]], writes=[r_Rb])
                        S.op(dve, lambda e: e.tensor_copy(out=negF[0:r, 0, :], in_=banks[6][0:r, 0:NH]),
                             reads=[r_bank[6], r_negF], writes=[r_negF])
                    else:
                        S.op(dve, lambda e: e.tensor_tensor(out=negF[0:r, t, :], in0=banks[6][0:r, 0:NH],
                                                            in1=Rb[0:r, t - 1, :], op=ALU.add),
                             reads=[r_bank[6], r_Rb, r_negF], writes=[r_negF])
                        S.op(dve, lambda e: e.tensor_tensor(out=Rb[:, t, :], in0=banks[5][:, 0:NH], in1=Rb[:, t - 1, :],
                                                            op=ALU.add),
                             reads=[r_bank[5], r_Rb], writes=[r_Rb])

                negFT = sb(sc, "negFT", [NH, L], F32)
                r_nFT = Res()
                for t, (r0, r) in enumerate(TOKT):
                    S.op(pe, lambda e: e.matmul(banks[7][0:NH, 0:r], lhsT=negF[0:r, t, :], rhs=id_f[0:r, 0:r],
                                                start=True, stop=True),
                         reads=[r_negF, r_c], writes=[r_bank[7]])
                    S.op(dve, lambda e: e.tensor_copy(out=negFT[:, r0:r0 + r], in_=banks[7][0:NH, 0:r]),
                         reads=[r_bank[7]], writes=[r_nFT])

                NB = 2
                wkv = [sb(sc, "wkv%d" % i, [128, 3, 8, 128], BF16) for i in range(NB)]
                r_wk = mkres(NB)
                QT = [sb(sc, "QT%d" % i, [128, SEQ], BF16) for i in range(NB)]
                KT = [sb(sc, "KT%d" % i, [128, L], BF16) for i in range(NB)]
                V = [sb(sc, "V%d" % i, [128, 17, 128], BF16) for i in range(NB)]
                r_Q = [mkres(4) for _ in range(NB)]
                r_K = [mkres(17) for _ in range(NB)]
                r_V = [mkres(17) for _ in range(NB)]
                sqb = Rot([(sb(sc, "sq%d" % i, [128, 512], BF16), Res()) for i in range(2)])
                rsb = Rot([(sb(sc, "rs%d" % i, [128, 2, 512], F32), Res()) for i in range(2)])
                ZZbuf = Rot([(sb(sc, "ZZ%d" % i, [128, 2, 512], F32), Res()) for i in range(2)])
                Abuf = Rot([(sb(sc, "A%d" % i, [128, 2, 512], BF16), Res()) for i in range(2)])
                FBbuf = [(sb(sc, "FB%d" % i, [128, 2, 512], F32), Res()) for i in range(2)]
                FKbuf = Rot([(sb(sc, "FK%d" % i, [128, 2, 512], F32), Res()) for i in range(3)])
                rden = Rot([(sb(sc, "rden%d" % i, [128, 512], F32), Res()) for i in range(2)])
                r_zas = mkres(2)
                NDUMMY_FOX = 0
                wkvv = wkvf_d.rearrange("(kc p) n -> p kc n", p=128)
                wqbv = wqb_d[0].rearrange("(kc p) n -> p kc n", p=128)

                def load_pair_w1(j):
                    jb = j % NB
                    S.dma(pool, wkv[jb][:, 0], wqbv[:, :, j * 128:(j + 1) * 128], writes=[r_wk[jb]])
                    S.dma(pool, wkv[jb][:, 1], wkvv[:, :, j * 128:(j + 1) * 128], writes=[r_wk[jb]])
                    S.dma(pool, wkv[jb][:, 2], wkvv[:, :, D + j * 128:D + (j + 1) * 128], writes=[r_wk[jb]])

                load_pair_w1(0)
                pbank = Rot([6, 7])
                sbank = Rot([4, 5])
                QCOL = [(512 * i, 512) for i in range(4)]
                for j in range(8):
                    jb = j % NB
                    if j + 1 < 8:
                        load_pair_w1(j + 1)

                    def proj_norm(i, src, r_src_of, c0, n, dst, rdst, nvec):
                        bk = pbank.next()
                        for kc in range(8):
                            S.op(pe, lambda e, kc=kc: e.matmul(banks[bk][:, 0:n], lhsT=wkv[jb][:, i, kc, :],
                                                               rhs=src[:, kc, c0:c0 + n], start=(kc == 0), stop=(kc == 7)),
                                 reads=[r_wk[jb]] + r_src_of, writes=[r_bank[bk]], signal=(kc == 7))
                        sqt, r_sq = sqb.next()
                        S.op(act, lambda e: e.activation(out=sqt[:, 0:n], in_=banks[bk][:, 0:n], func=AF.Square),
                             reads=[r_bank[bk]], writes=[r_sq])
                        bs = sbank.next()
                        S.op(pe, lambda e: e.matmul(banks[bs][:, 0:n], lhsT=blk_b, rhs=sqt[:, 0:n], start=True, stop=True),
                             reads=[r_sq, r_c], writes=[r_bank[bs]])
                        rst, r_rs = rsb.next()
                        S.op(act, lambda e: e.activation(out=rst[:, 0, 0:n], in_=banks[bs][:, 0:n], func=AF.Ln,
                                                         scale=1.0 / HD, bias=EPS),
                             reads=[r_bank[bs]], writes=[r_rs])
                        S.op(act, lambda e: e.activation(out=rst[:, 1, 0:n], in_=rst[:, 0, 0:n], func=AF.Exp, scale=-0.5),
                             reads=[r_rs], writes=[r_rs])
                        S.op(dve, lambda e: e.scalar_tensor_tensor(out=dst, in0=banks[bk][:, 0:n], scalar=nvec[:, 0:1],
                                                                   in1=rst[:, 1, 0:n], op0=ALU.mult, op1=ALU.mult),
                             reads=[r_bank[bk], r_rs, r_par], writes=rdst)

                    for ci, (c0, n) in enumerate(COLT):
                        tl = tiles_of(c0, n)
                        proj_norm(1, xkT, [r_xk[t] for t in tl], c0, n, KT[jb][:, c0:c0 + n], [r_K[jb][t] for t in tl], knorm2)
                    for ci, (c0, n) in enumerate(QCOL):
                        tl = [1 + 4 * ci + i for i in range(4)]
                        proj_norm(0, xqT, [r_xq[t] for t in tl], c0, n, QT[jb][:, c0:c0 + n], [r_Q[jb][ci]], qnorm2)
                    for t, (r0, r) in enumerate(TOKT):
                        bk = pbank.next()
                        for kc in range(8):
                            S.op(pe, lambda e, kc=kc, bk=bk: e.matmul(
                                banks[bk][0:r, 0:128], lhsT=xkT[:, kc, r0:r0 + r], rhs=wkv[jb][:, 2, kc, :],
                                start=(kc == 0), stop=(kc == 7)),
                                 reads=[r_wk[jb], r_xk[t]], writes=[r_bank[bk]], signal=(kc == 7))
                        S.op(dve, lambda e, bk=bk: e.tensor_copy(out=V[jb][0:r, t, :], in_=banks[bk][0:r, 0:128]),
                             reads=[r_bank[bk]], writes=[r_V[jb][t]])
                    blocks = []
                    for qi, (qc0, n) in enumerate(QCOL):
                        kts = [0] + [t for t in range(1, 17) if TOKT[t][0] - NM < qc0 + n]
                        for bi, kt in enumerate(kts):
                            kc0, r = TOKT[kt]
                            kq = kc0 - NM
                            qlo = max(0, kq - qc0)
                            masked = (kq + r - 1 >= qc0 + qlo)
                            blocks.append(dict(qi=qi, qc0=qc0, n=n, kt=kt, kc0=kc0, r=r, qlo=qlo, masked=masked,
                                               kq=kq, first=(bi == 0), last=(bi == len(kts) - 1)))
                    HP = (slice(0, 64), slice(64, 128))

                    def make_fb(qi):
                        qc0_, n_ = QCOL[qi]
                        FBt, r_FB = FBbuf[qi % 2]
                        for hh in range(2):
                            S.op(pe, lambda e: e.matmul(banks[6][:, :], lhsT=SelAll[:, 2 * j + hh, :],
                                                        rhs=negFT[:, NM + qc0_:NM + qc0_ + n_], start=True, stop=True),
                                 reads=[r_nFT, r_par], writes=[r_bank[6]])
                            S.op(dve, lambda e: e.tensor_scalar(out=FBt[:, hh, :], in0=banks[6][:, :], scalar1=-1.0,
                                                                scalar2=None, op0=ALU.mult),
                                 reads=[r_bank[6]], writes=[r_FB])

                    for bi_, b_ in enumerate(blocks):
                        b_["zs"] = bi_ % 2

                    def f1(b):
                        r, qlo, n, kc0, qc0 = b["r"], b["qlo"], b["n"], b["kc0"], b["qc0"]
                        zs = b["zs"]
                        for hh in range(2):
                            S.op(pe, lambda e: e.matmul(PS[0:r, 2 * zs + hh, qlo:n], lhsT=KT[jb][HP[hh], kc0:kc0 + r],
                                                        rhs=QT[jb][HP[hh], qc0 + qlo:qc0 + n], start=True, stop=True),
                                 reads=[r_K[jb][b["kt"]], r_Q[jb][b["qi"]]], writes=[r_zas[zs]], signal=(hh == 1))

                    def f0(b):
                        r, qlo, n = b["r"], b["qlo"], b["n"]
                        if b["first"]:
                            if b["qi"] == 0:
                                make_fb(0)
                            if b["qi"] + 1 < len(QCOL):
                                make_fb(b["qi"] + 1)
                        FBt, r_FB = FBbuf[b["qi"] % 2]
                        FKt, r_FK = FKbuf.next()
                        b["FK"], b["r_FK"] = FKt, r_FK
                        for hh in range(2):
                            h = 2 * j + hh
                            S.op(pool, lambda e: e.tensor_scalar(out=FKt[0:r, hh, qlo:n], in0=FBt[0:r, hh, qlo:n],
                                                                 scalar1=negF[0:r, b["kt"], h:h + 1], scalar2=1.0,
                                                                 op0=ALU.add, op1=ALU.mult),
                                 reads=[r_FB, r_negF], writes=[r_FK])

                    def f1b(b):
                        r, qlo, n = b["r"], b["qlo"], b["n"]
                        zs = b["zs"]
                        FKt, r_FK = b["FK"], b["r_FK"]
                        ZZt, r_ZZ = ZZbuf.next()
                        b["ZZ"], b["r_ZZ"] = ZZt, r_ZZ
                        S.op(dve, lambda e: e.tensor_tensor(out=ZZt[0:r, :, qlo:n], in0=PS[0:r, 2 * zs:2 * zs + 2, qlo:n],
                                                            in1=FKt[0:r, :, qlo:n], op=ALU.add),
                             reads=[r_zas[zs], r_FK], writes=[r_ZZ])

                    def f2(b):
                        r, qlo, n, qc0, kq = b["r"], b["qlo"], b["n"], b["qc0"], b["kq"]
                        ZZt, r_ZZ = b["ZZ"], b["r_ZZ"]
                        At, r_A = Abuf.next()
                        b["A"], b["r_A"] = At, r_A
                        S.op(act, lambda e: e.activation(out=At[0:r, :, qlo:n], in_=ZZt[0:r, :, qlo:n], func=AF.Exp),
                             reads=[r_ZZ], writes=[r_A])
                        if b["masked"]:
                            S.op(pool, lambda e: e.affine_select(out=At[0:r, :, qlo:qlo + 128], in_=At[0:r, :, qlo:qlo + 128],
                                                                 pattern=[[0, 2], [1, 128]], compare_op=ALU.is_ge, fill=0.0,
                                                                 base=qc0 + qlo - kq, channel_multiplier=-1),
                                 reads=[r_A], writes=[r_A])

                    def f3(b):
                        bn, bd = 4, 5
                        r, qlo, n, qc0 = b["r"], b["qlo"], b["n"], b["qc0"]
                        At, r_A = b["A"], b["r_A"]
                        for _ in range(NDUMMY_FOX):
                            S.op(pe, lambda e: e.matmul(banks[7][:, :], lhsT=cb[:, 0:128], rhs=KT[jb][:, 0:512],
                                                        start=True, stop=True),
                                 reads=[r_c] + [r_K[jb][t] for t in range(5)], writes=[r_bank[7]], signal=False)
                        for hh in range(2):
                            S.op(pe, lambda e: e.matmul(PS[HP[hh], bn, qlo:n], lhsT=V[jb][0:r, b["kt"], HP[hh]],
                                                        rhs=At[0:r, hh, qlo:n], start=b["first"], stop=b["last"],
                                                        skip_group_check=True),
                                 reads=[r_V[jb][b["kt"]], r_A], writes=[r_bank[bn]], signal=(hh == 1))
                        for hh in range(2):
                            S.op(pe, lambda e: e.matmul(PS[HP[hh], bd, qlo:n], lhsT=ones_b[0:r, 0:64],
                                                        rhs=At[0:r, hh, qlo:n], start=b["first"], stop=b["last"],
                                                        skip_group_check=True),
                                 reads=[r_A, r_c], writes=[r_bank[bd]], signal=(hh == 1))
                        if b["last"]:
                            rd, r_rd = rden.next()
                            S.op(dve, lambda e: e.reciprocal(out=rd[:, 0:n], in_=PS[:, bd, 0:n]),
                                 reads=[r_bank[bd]], writes=[r_rd])
                            S.op(dve, lambda e: e.tensor_tensor(out=oT[:, j, NM + qc0:NM + qc0 + n], in0=PS[:, bn, 0:n],
                                                                in1=rd[:, 0:n], op=ALU.mult),
                                 reads=[r_bank[bn], r_rd], writes=[r_oT[t] for t in tiles_of(NM + qc0, n)])

                    pipeline_rev(blocks, [f0, f1, f1b, f2, f3])
            S.barrier()
            if stop_after <= 4:
                continue

            with ExitStack() as sd:
                y = sb(sd, "y2", [128, 16, D], F32)
                r_y = mkres(16)
                comb = sb(sd, "comb", [128, 16, NE], F32)
                r_comb = mkres(16)
                with ExitStack() as s5:
                    wo = sb(s5, "wob", [128, 8, D], BF16)
                    r_wo = Res()
                    S.dma(pool, wo[:], wob_d[0].rearrange("(kc p) n -> p kc n", p=128), writes=[r_wo])
                    gb = sb(s5, "gbm", [128, D], F32)
                    r_gb = Res()
                    load_gb(gb, r_gb, g_fm_d)
                    hbuf = [sb(s5, "hb%d" % i, [128, D], F32) for i in range(4)]
                    r_hb = mkres(4)
                    nw = NormWork(s5, "n5", nbuf=3, want32=True)
                    xhl = [(sb(s5, "xh%d" % i, [128, D], BF16), sb(s5, "xl%d" % i, [128, D], BF16)) for i in range(2)]
                    r_xh, r_xl = mkres(2), mkres(2)
                    xlT = [sb(s5, "xlT%d" % i, [128, 8, 128], BF16) for i in range(2)]
                    r_xlT = mkres(2)
                    rt = [sb(s5, "rt%d" % i, [128, 5, NE], F32) for i in range(2)]
                    r_rt = mkres(2)
                    obank = Rot([0, 1, 2, 3])
                    items = [dict(t=t, r0=TOKT[t][0], yt=t - 1, k3=t % 4, k2=(t - 1) % 2) for t in range(1, 17)]

                    def p5_0(it):
                        t, r0, yt, k3 = it["t"], it["r0"], it["yt"], it["k3"]
                        S.dma(sp, hbuf[k3][:, :], h2_d[s, r0:r0 + 128, :], writes=[r_hb[k3]])
                        for half in range(2):
                            bk = obank.next()
                            for jj in range(8):
                                S.op(pe, lambda e, jj=jj, bk=bk: e.matmul(
                                    banks[bk][:, :], lhsT=oT[:, jj, r0:r0 + 128], rhs=wo[:, jj, half * 512:(half + 1) * 512],
                                    start=(jj == 0), stop=(jj == 7)),
                                     reads=[r_oT[t], r_wo], writes=[r_bank[bk]], signal=(jj == 7))
                            S.op(dve, lambda e, bk=bk: e.tensor_tensor(
                                out=y[:, yt, half * 512:(half + 1) * 512], in0=banks[bk][:, :],
                                in1=hbuf[k3][:, half * 512:(half + 1) * 512], op=ALU.add),
                                 reads=[r_bank[bk], r_hb[k3]], writes=[r_y[yt]])
                        if debug:
                            S.dma(sp, dbg["h3"][s, yt * 128:(yt + 1) * 128, :], y[:, yt, :], reads=[r_y[yt]])

                    def p5_1(it):
                        yt = it["yt"]
                        it["k"], it["rstd"], it["r_rs"] = norm_stats(nw, y[:, yt, :], 128, r_y[yt])

                    def p5_2(it):
                        yt, k2, k = it["yt"], it["k2"], it["k"]
                        rx = nw.res[k][2]
                        xs = nw.xs[k]
                        S.op(dve, lambda e: e.scalar_tensor_tensor(out=xs[:, :], in0=y[:, yt, :], scalar=it["rstd"], in1=gb[:, :],
                                                                   op0=ALU.mult, op1=ALU.mult),
                             reads=[r_y[yt], it["r_rs"], r_gb], writes=[rx])
                        xh, xl = xhl[k2]
                        S.op(act, lambda e: e.activation(out=xh[:, :], in_=xs[:, :], func=AF.Copy),
                             reads=[rx], writes=[r_xh[k2]])
                        S.op(dve, lambda e: e.tensor_tensor(out=xl[:, :], in0=xs[:, :], in1=xh[:, :], op=ALU.subtract),
                             reads=[rx, r_xh[k2]], writes=[r_xl[k2]])
                        for (srcx, r_srcx, bk) in ((xh, r_xh[k2], 4), (xl, r_xl[k2], 5)):
                            psT = banks[bk][:, :].bitcast(BF16).rearrange("p (c n) -> p c n", c=8)
                            for c in range(8):
                                S.op(pe, lambda e, c=c: e.transpose(out=psT[:, c, :], in_=srcx[:, c * 128:(c + 1) * 128],
                                                                    identity=id_b),
                                     reads=[r_srcx, r_c], writes=[r_bank[bk]], signal=(c == 7))

                    def p5_3(it):
                        t, r0, k2 = it["t"], it["r0"], it["k2"]
                        psT = banks[4][:, :].bitcast(BF16).rearrange("p (c n) -> p c n", c=8)
                        S.op(act, lambda e: e.activation(out=oT[:, :, r0:r0 + 128], in_=psT, func=AF.Copy),
                             reads=[r_bank[4]], writes=[r_oT[t]])
                        psT = banks[5][:, :].bitcast(BF16).rearrange("p (c n) -> p c n", c=8)
                        S.op(dve, lambda e: e.tensor_copy(out=xlT[k2][:, :, :], in_=psT),
                             reads=[r_bank[5]], writes=[r_xlT[k2]])
                        terms = [(oT, r_oT[t], r0, 0), (xlT[k2], r_xlT[k2], 0, 0), (oT, r_oT[t], r0, 1)]
                        for ti, (lt, r_lt, c0_, wi) in enumerate(terms):
                            for kc in range(8):
                                S.op(pe, lambda e, kc=kc: e.matmul(banks[6][:, 0:NE], lhsT=lt[:, kc, c0_:c0_ + 128],
                                                                   rhs=wrs[:, wi, kc, :],
                                                                   start=(ti == 0 and kc == 0), stop=(ti == 2 and kc == 7)),
                                     reads=[r_lt, r_par], writes=[r_bank[6]], signal=(ti == 2 and kc == 7))

                    def p5_4(it):
                        yt, k2 = it["yt"], it["k2"]
                        R_, rr_ = rt[k2], r_rt[k2]
                        S.op(dve, lambda e: e.tensor_copy(out=R_[:, 0, :], in_=banks[6][:, 0:NE]),
                             reads=[r_bank[6]], writes=[rr_])
                        S.op(dve, lambda e: e.max(out=R_[:, 1, :], in_=R_[:, 0, :]), reads=[rr_], writes=[rr_])
                        S.op(dve, lambda e: e.tensor_scalar(out=R_[:, 2, 0:1], in0=R_[:, 1, 0:1], scalar1=-1.0, scalar2=None,
                                                            op0=ALU.mult), reads=[rr_], writes=[rr_])
                        S.op(act, lambda e: e.activation(out=R_[:, 3, :], in_=R_[:, 0, :], func=AF.Exp, bias=R_[:, 2, 0:1]),
                             reads=[rr_], writes=[rr_])
                        S.op(dve, lambda e: e.scalar_tensor_tensor(out=R_[:, 4, :], in0=R_[:, 0, :], scalar=R_[:, 1, 1:2],
                                                                   in1=R_[:, 3, :], op0=ALU.is_ge, op1=ALU.mult,
                                                                   accum_out=R_[:, 2, 1:2]),
                             reads=[rr_], writes=[rr_])
                        S.op(dve, lambda e: e.reciprocal(out=R_[:, 2, 2:3], in_=R_[:, 2, 1:2]), reads=[rr_], writes=[rr_])
                        S.op(dve, lambda e: e.tensor_scalar(out=comb[:, yt, :], in0=R_[:, 4, :], scalar1=R_[:, 2, 2:3],
                                                            scalar2=None, op0=ALU.mult),
                             reads=[rr_], writes=[r_comb[yt]])
                        if debug:
                            S.dma(sp, dbg["comb"][s, yt * 128:(yt + 1) * 128, :], comb[:, yt, :], reads=[r_comb[yt]])

                    pipeline_rev(items, [p5_0, p5_1, p5_2, p5_3, p5_4])
                S.barrier()
                if stop_after >= 6:
                    with ExitStack() as s6:
                        srcs = []
                        for ex in range(NE):
                            for (f0, nfc) in UNITS:
                                srcs.append((wgum_d[0, ex][:, f0 * 128:(f0 + nfc) * 128],
                                             wgum_d[0, ex][:, DFF + f0 * 128:DFF + (f0 + nfc) * 128],
                                             wdm_d[0, ex][f0 * 128:(f0 + nfc) * 128, :], nfc, ex))
                        ffn_units(s6, oT, r_oT, COLT[1:], y, r_y, lambda t: t - 1, srcs,
                                  lambda key, yt, r: (comb[0:r, yt, key:key + 1], r_comb[yt]))
                for yt in range(16):
                    S.dma(sp, out_d[s, yt * 128:(yt + 1) * 128, :], y[:, yt, :], reads=[r_y[yt]])
                S.barrier()
    S.barrier()


_PROG = {}


def _in_map(inputs, b0, nseq):
    f = lambda a: np.ascontiguousarray(np.asarray(a, dtype=np.float32))
    return {
        "x": f(inputs["x"][b0:b0 + nseq]),
        "meta_tokens": f(inputs["meta_tokens"]),
        "norm_attn_a": f(inputs["norm_attn_a"]),
        "w_qkv_a": f(inputs["w_qkv_a"]),
        "w_o_a": f(inputs["w_o_a"]),
        "norm_kv": f(inputs["norm_kv"]).reshape(1, D),
        "w_kvf": f(inputs["w_kvf"]),
        "b_f": f(inputs["b_f"]).reshape(1, NH),
        "k_norm": f(inputs["k_norm"]).reshape(HD, 1),
        "norm_attn_b": f(inputs["norm_attn_b"]),
        "w_q_b": f(inputs["w_q_b"]),
        "q_norm_b": f(inputs["q_norm_b"]).reshape(HD, 1),
        "w_o_b": f(inputs["w_o_b"]),
        "norm_ffn_dense": f(inputs["norm_ffn_dense"]),
        "w_gu_dense": f(inputs["w_gu_dense"]),
        "w_down_dense": f(inputs["w_down_dense"]),
        "norm_ffn_moe": f(inputs["norm_ffn_moe"]),
        "w_router": f(inputs["w_router"]),
        "w_gu_moe": f(inputs["w_gu_moe"]),
        "w_down_moe": f(inputs["w_down_moe"]),
        "consts": make_consts(),
    }


def kernel(**inputs):
    nseq = 2
    nc = build_program(nseq=nseq)
    in_maps = [_in_map(inputs, nseq * c, nseq) for c in range(NCORES)]
    res = run_bass_kernel_spmd(nc, in_maps, core_ids=list(range(NCORES)))
    out = np.concatenate([np.asarray(r["out"]) for r in res.results], axis=0)
    return out.astype(np.float32)
```

```python
import bisect
from contextlib import ExitStack

import numpy as np
import concourse.bass as bass
import concourse.mybir as mybir
from concourse.bass_utils import run_bass_kernel_spmd

F32 = mybir.dt.float32
BF16 = mybir.dt.bfloat16
AF = mybir.ActivationFunctionType
ALU = mybir.AluOpType

D = 1024
NM = 16
SEQ = 2048
L = NM + SEQ
NH = 16
HD = 64
DFF = 2816
NE = 8
EPS = 1e-6
NCORES = 8

COLT = [(0, 16)] + [(16 + 512 * i, 512) for i in range(4)]
TOKT = [(0, 16)] + [(16 + 128 * i, 128) for i in range(16)]
UNITS = [(0, 4), (4, 4), (8, 4), (12, 4), (16, 3), (19, 3)]

C_ID, C_TRINEG, C_ONES, C_TRILE, C_BLK, C_NEG1, C_W = 0, 128, 256, 384, 512, 640, 768


def make_consts():
    c = np.zeros((128, C_W), np.float32)
    j = np.arange(128)[:, None]
    s = np.arange(128)[None, :]
    c[:, C_ID:C_ID + 128] = (j == s)
    c[:, C_TRINEG:C_TRINEG + 128] = -1.0 * (j >= s)
    c[:, C_ONES:C_ONES + 128] = 1.0
    c[:, C_TRILE:C_TRILE + 128] = 1.0 * (j <= s)
    c[:, C_BLK:C_BLK + 128] = 1.0 * ((j // 64) == (s // 64))
    c[:, C_NEG1:C_NEG1 + 128] = -1.0
    return c


class Res:
    __slots__ = ("w", "r", "gen")

    def __init__(self):
        self.w = None
        self.r = {}
        self.gen = -1


def mkres(n):
    return [Res() for _ in range(n)]


class Eng:
    def __init__(self, name, h):
        self.name = name
        self.h = h
        self.seq = 0
        self.sigs_seq = []
        self.sigs_ev = []
        self.sem = None
        self.cnt = 0
        self.waited = {}


class Sched:
    def __init__(self, nc, stack, n_dma_sems=48):
        self.nc = nc
        self.stack = stack
        self.sems = []
        self.pe = Eng("pe", nc.tensor)
        self.act = Eng("act", nc.scalar)
        self.dve = Eng("dve", nc.vector)
        self.pool = Eng("pool", nc.gpsimd)
        self.sp = Eng("sp", nc.sync)
        self.engs = [self.pe, self.act, self.dve, self.pool, self.sp]
        self.dmas_hw = [[self.new_sem("dqh%d" % i), 0] for i in range(n_dma_sems // 2)]
        self.dmas_sw = [[self.new_sem("dqs%d" % i), 0] for i in range(n_dma_sems // 2)]
        self.dmas = self.dmas_hw + self.dmas_sw
        self.rr_hw = 0
        self.rr_sw = 0
        self.gen = 0

    def new_sem(self, name):
        h = self.stack.enter_context(self.nc.semaphore(name))
        self.sems.append(h)
        return len(self.sems) - 1

    def _sig(self, E):
        if E.sem is None or E.cnt >= 30000:
            E.sem = self.new_sem("%s_e%d" % (E.name, len(self.sems)))
            E.cnt = 0
        E.cnt += 1
        return (E.sem, E.cnt)

    def _resolve(self, tok):
        if tok[0] == "d":
            return (tok[1], tok[2])
        E, seq = tok[1], tok[2]
        i = bisect.bisect_left(E.sigs_seq, seq)
        assert i < len(E.sigs_seq), "unresolved dep on %s seq %d" % (E.name, seq)
        return E.sigs_ev[i]

    def _wait(self, E, ev):
        s, v = ev
        if E.waited.get(s, 0) >= v:
            return
        E.h.wait_ge(self.sems[s], v)
        E.waited[s] = v

    def _deps(self, E, reads, writes):
        toks = []
        for r in reads:
            if r.gen == self.gen and r.w is not None:
                toks.append((r.w, 0))
        for w in writes:
            if w.gen == self.gen:
                if w.w is not None:
                    toks.append((w.w, 1))
                for t in w.r.values():
                    toks.append((t, 2))
        for tok, kind in toks:
            if tok[0] == "c" and tok[1] is E:
                if E is self.pe or kind == 2:
                    continue
            self._wait(E, self._resolve(tok))

    def _update(self, tok, reads, writes):
        key = tok[1] if tok[0] == "c" else ("d", tok[1])
        for r in reads:
            if r.gen != self.gen:
                r.gen = self.gen
                r.w = None
                r.r = {}
            r.r[key] = tok
        for w in writes:
            w.gen = self.gen
            w.w = tok
            w.r = {}

    def op(self, E, fn, reads=(), writes=(), signal=True):
        self._deps(E, reads, writes)
        ins = fn(E.h)
        E.seq += 1
        if signal:
            ev = self._sig(E)
            ins.then_inc(self.sems[ev[0]], 1)
            E.sigs_seq.append(E.seq)
            E.sigs_ev.append(ev)
        self._update(("c", E, E.seq), reads, writes)

    def dma(self, Q, out, in_, reads=(), writes=(), **kw):
        self._deps(Q, reads, writes)
        if Q is self.pool:
            slot = self.dmas_sw[self.rr_sw]
            self.rr_sw = (self.rr_sw + 1) % len(self.dmas_sw)
        else:
            slot = self.dmas_hw[self.rr_hw]
            self.rr_hw = (self.rr_hw + 1) % len(self.dmas_hw)
        self._wait(Q, (slot[0], slot[1]))
        slot[1] += 16
        Q.h.dma_start(out=out, in_=in_, **kw).then_inc(self.sems[slot[0]], 16)
        self._update(("d", slot[0], slot[1]), reads, writes)

    def barrier(self):
        evs = []
        for E in self.engs:
            if E.sigs_ev:
                assert E.sigs_seq[-1] == E.seq, "last instr of %s does not signal" % E.name
                evs.append(E.sigs_ev[-1])
        for s, v in self.dmas:
            if v:
                evs.append((s, v))
        for E in self.engs:
            for ev in evs:
                self._wait(E, ev)
        self.gen += 1


def pipeline(blocks, stages):
    n, k = len(blocks), len(stages)
    for step in range(n + k - 1):
        for si, st in enumerate(stages):
            i = step - si
            if 0 <= i < n:
                st(blocks[i])


def pipeline_rev(blocks, stages):
    n, k = len(blocks), len(stages)
    for step in range(n + k - 1):
        for si in range(k - 1, -1, -1):
            i = step - si
            if 0 <= i < n:
                stages[si](blocks[i])


class Rot:
    def __init__(self, items):
        self.items = items
        self.i = 0

    def next(self):
        it = self.items[self.i % len(self.items)]
        self.i += 1
        return it


def build_program(nseq=2, stop_after=99, debug=False):
    nc = bass.Bass("TRN2", target_bir_lowering=False)
    top = ExitStack()
    with top:
        _build(nc, top, nseq, stop_after, debug)
    return nc


def _build(nc, top, nseq, stop_after, debug):
    def din(name, shape):
        return nc.dram_tensor(name, list(shape), F32, kind="ExternalInput").ap()

    x_d = din("x", [nseq, SEQ, D])
    meta_d = din("meta_tokens", [NM, D])
    g_a_d = din("norm_attn_a", [1, D])
    wqkv_d = din("w_qkv_a", [1, D, 3 * D])
    woa_d = din("w_o_a", [1, D, D])
    g_kv_d = din("norm_kv", [1, D])
    wkvf_d = din("w_kvf", [D, 2 * D + NH])
    bf_d = din("b_f", [1, NH])
    kn_d = din("k_norm", [HD, 1])
    g_b_d = din("norm_attn_b", [1, D])
    wqb_d = din("w_q_b", [1, D, D])
    qn_d = din("q_norm_b", [HD, 1])
    wob_d = din("w_o_b", [1, D, D])
    g_fd_d = din("norm_ffn_dense", [1, D])
    wgud_d = din("w_gu_dense", [1, D, 2 * DFF])
    wdd_d = din("w_down_dense", [1, DFF, D])
    g_fm_d = din("norm_ffn_moe", [1, D])
    wr_d = din("w_router", [1, D, NE])
    wgum_d = din("w_gu_moe", [1, NE, D, 2 * DFF])
    wdm_d = din("w_down_moe", [1, NE, DFF, D])
    consts_d = din("consts", [128, C_W])
    out_d = nc.dram_tensor("out", [nseq, SEQ, D], F32, kind="ExternalOutput").ap()
    h2_d = nc.dram_tensor("h2s", [nseq, L, D], F32, kind="ExternalOutput" if debug else "Internal").ap()
    dbg = {}
    if debug:
        dbg["h1"] = nc.dram_tensor("dbg_h1", [nseq, L, D], F32, kind="ExternalOutput").ap()
        dbg["h3"] = nc.dram_tensor("dbg_h3", [nseq, SEQ, D], F32, kind="ExternalOutput").ap()
        dbg["comb"] = nc.dram_tensor("dbg_comb", [nseq, SEQ, NE], F32, kind="ExternalOutput").ap()

    S = Sched(nc, top)
    pe, act, dve, pool, sp = S.pe, S.act, S.dve, S.pool, S.sp

    uid = [0]

    def sb(stack, name, shape, dt):
        uid[0] += 1
        return stack.enter_context(nc.sbuf_tensor("%s_u%d" % (name, uid[0]), list(shape), dt))

    cf = sb(top, "cf", [128, C_W], F32)
    cb = sb(top, "cb", [128, C_W], BF16)
    r_c = Res()
    S.dma(sp, cf[:], consts_d[:, :], writes=[r_c])
    S.dma(pool, cb[:], consts_d[:, :], writes=[r_c])
    id_f = cf[:, C_ID:C_ID + 128]
    ones_f = cf[:, C_ONES:C_ONES + 128]
    trile_f = cf[:, C_TRILE:C_TRILE + 128]
    id_b = cb[:, C_ID:C_ID + 128]
    trineg_b = cb[:, C_TRINEG:C_TRINEG + 128]
    ones_b = cb[:, C_ONES:C_ONES + 128]
    blk_b = cb[:, C_BLK:C_BLK + 128]
    neg1_b = cb[:, C_NEG1:C_NEG1 + 128]

    PS = top.enter_context(nc.psum_tensor("ps_all", [128, 8, 512], F32))
    banks = [PS[:, i, :] for i in range(8)]
    r_bank = mkres(8)

    knorm2 = sb(top, "knorm2", [128, 1], F32)
    qnorm2 = sb(top, "qnorm2", [128, 1], F32)
    bfb = sb(top, "bfb", [128, NH], F32)
    wr32 = sb(top, "wr32", [128, 8, NE], F32)
    r_par = Res()
    for hh in range(2):
        S.dma(sp, knorm2[hh * 64:(hh + 1) * 64, :], kn_d[:, :], writes=[r_par])
        S.dma(sp, qnorm2[hh * 64:(hh + 1) * 64, :], qn_d[:, :], writes=[r_par])
    S.dma(sp, bfb[:], bf_d.partition_broadcast(128), writes=[r_par])
    S.dma(sp, wr32[:], wr_d[0].rearrange("(kc p) e -> p kc e", p=128), writes=[r_par])
    S.op(dve, lambda e: e.tensor_scalar(out=qnorm2[:], in0=qnorm2[:], scalar1=HD ** -0.5, scalar2=None, op0=ALU.mult),
         reads=[r_par], writes=[r_par])
    wrs = sb(top, "wrs", [128, 2, 8, NE], BF16)
    S.op(dve, lambda e: e.tensor_copy(out=wrs[:, 0], in_=wr32[:]), reads=[r_par], writes=[r_par])
    S.op(dve, lambda e: e.tensor_tensor(out=wrs[:, 1], in0=wr32[:], in1=wrs[:, 0], op=ALU.subtract),
         reads=[r_par], writes=[r_par])

    SelAll = sb(top, "SelAll", [NH, NH, 128], F32)
    S.op(dve, lambda e: e.tensor_copy(out=SelAll[:, :, :], in_=cf[0:NH, C_ID:C_ID + NH].unsqueeze(2).broadcast_to([NH, NH, 128])),
         reads=[r_c], writes=[r_par])

    def tiles_of(c0, n):
        return [t for t, (r0, r) in enumerate(TOKT) if r0 < c0 + n and r0 + r > c0]

    class NormWork:
        def __init__(self, stack, tag, nbuf=2, want32=False):
            self.n = nbuf
            self.junk = [sb(stack, "%s_junk%d" % (tag, i), [128, D], BF16) for i in range(nbuf)]
            self.ss = [sb(stack, "%s_ss%d" % (tag, i), [128, 4], F32) for i in range(nbuf)]
            self.xs = [sb(stack, "%s_xs%d" % (tag, i), [128, D], F32 if want32 else BF16) for i in range(nbuf)]
            self.res = [mkres(3) for _ in range(nbuf)]
            self.i = 0

    def norm_stats(nw, h_ap, r, r_h):
        k = nw.i % nw.n
        nw.i += 1
        rj, rs, rx = nw.res[k]
        ss = nw.ss[k]
        S.op(act, lambda e: e.activation(out=nw.junk[k][0:r, :], in_=h_ap, func=AF.Square, accum_out=ss[0:r, 0:1]),
             reads=[r_h], writes=[rj, rs])
        S.op(act, lambda e: e.activation(out=ss[0:r, 1:2], in_=ss[0:r, 0:1], func=AF.Ln, scale=1.0 / D, bias=EPS),
             reads=[rs], writes=[rs])
        S.op(act, lambda e: e.activation(out=ss[0:r, 2:3], in_=ss[0:r, 1:2], func=AF.Exp, scale=-0.5),
             reads=[rs], writes=[rs])
        return k, ss[0:r, 2:3], rs

    def norm_T_a(nw, k, rstd, r_rs, h_ap, r, r_h, gb, r_gb, bank_i):
        rx = nw.res[k][2]
        xs = nw.xs[k]
        S.op(dve, lambda e: e.scalar_tensor_tensor(out=xs[0:r, :], in0=h_ap, scalar=rstd, in1=gb[0:r, :],
                                                   op0=ALU.mult, op1=ALU.mult),
             reads=[r_h, r_rs, r_gb], writes=[rx])
        psT = banks[bank_i][:, :].bitcast(BF16).rearrange("p (c n) -> p c n", c=8)
        for c in range(8):
            S.op(pe, lambda e, c=c: e.transpose(out=psT[:, c, 0:r], in_=xs[0:r, c * 128:(c + 1) * 128],
                                                identity=id_b[0:r, 0:r]),
                 reads=[rx, r_c], writes=[r_bank[bank_i]], signal=(c == 7))

    def norm_T_b(r, xT, col0, r_dst, bank_i, eng=None):
        psT = banks[bank_i][:, :].bitcast(BF16).rearrange("p (c n) -> p c n", c=8)
        S.op(dve, lambda e: e.tensor_copy(out=xT[:, :, col0:col0 + r], in_=psT[:, :, 0:r]),
             reads=[r_bank[bank_i]], writes=[r_dst])

    def norm_T(nw, k, rstd, r_rs, h_ap, r, r_h, gb, r_gb, xT, col0, r_dst, bank_i):
        norm_T_a(nw, k, rstd, r_rs, h_ap, r, r_h, gb, r_gb, bank_i)
        norm_T_b(r, xT, col0, r_dst, bank_i)

    def norm_pipeline(nw, items):
        def s0(it):
            if it.get("pre"):
                it["pre"]()

        def s1(it):
            it["k"], it["rstd"], it["r_rs"] = norm_stats(nw, it["h"], it["r"], it["r_h"])

        def s2(it):
            for oi, (gb_, r_gb_, xT_, col0_, r_dst_, bank_) in enumerate(it["outs"]):
                k = it["k"]
                if oi > 0:
                    k = nw.i % nw.n
                    nw.i += 1
                norm_T_a(nw, k, it["rstd"], it["r_rs"], it["h"], it["r"], it["r_h"], gb_, r_gb_, bank_)

        def s3(it):
            for (gb_, r_gb_, xT_, col0_, r_dst_, bank_) in it["outs"]:
                norm_T_b(it["r"], xT_, col0_, r_dst_, bank_)
            if it.get("post"):
                it["post"]()

        pipeline_rev(items, [s0, s1, s2, s3])

    def load_gb(gb, r_gb, g_dram):
        S.dma(sp, gb[:], g_dram.partition_broadcast(128), writes=[r_gb])

    def h0_src(s, t):
        r0, r = TOKT[t]
        if t == 0:
            return meta_d[:, :]
        return x_d[s, r0 - NM:r0 - NM + r, :]

    def ffn_units(stack, xT, r_xT, coltiles, y, r_y, ytile_of, unit_srcs, scale_of, NW=3, on_tile_done=None):
        wg = [sb(stack, "wg%d" % i, [128, 8, 512], BF16) for i in range(NW)]
        wu = [sb(stack, "wu%d" % i, [128, 8, 512], BF16) for i in range(NW)]
        wd = [sb(stack, "wd%d" % i, [128, 4, D], BF16) for i in range(NW)]
        r_w = mkres(NW)
        G = [sb(stack, "G%d" % i, [128, 4, 512], BF16) for i in range(2)]
        r_G = mkres(2)
        sg = Rot([(sb(stack, "sg%d" % i, [128, 512], BF16), Res()) for i in range(3)])
        gu_banks = Rot([0, 1, 2, 3])
        y_banks = Rot([4, 5, 6, 7])

        def load_unit(u):
            gd, ud, dd, nfc, _ = unit_srcs[u]
            k = u % NW
            fw = nfc * 128
            S.dma(pool, wg[k][:, :, 0:fw], gd.rearrange("(kc p) n -> p kc n", p=128), writes=[r_w[k]])
            S.dma(pool, wu[k][:, :, 0:fw], ud.rearrange("(kc p) n -> p kc n", p=128), writes=[r_w[k]])
            S.dma(pool, wd[k][:, 0:nfc, :], dd.rearrange("(fc p) n -> p fc n", p=128), writes=[r_w[k]])

        jobs = []
        for u in range(len(unit_srcs)):
            for ci, (c0, n) in enumerate(coltiles):
                jobs.append(dict(u=u, ci=ci, c0=c0, n=n, idx=len(jobs)))

        def stage_a(jb):
            u, c0, n = jb["u"], jb["c0"], jb["n"]
            if jb["ci"] == 0 and u == 0:
                load_unit(0)
                if len(unit_srcs) > 1:
                    load_unit(1)
            if jb["ci"] == 1 and u + 2 < len(unit_srcs):
                load_unit(u + 2)
            k = u % NW
            nfc = unit_srcs[u][3]
            gi = jb["idx"] % 2
            rx = [r_xT[t] for t in tiles_of(c0, n)]
            for fc in range(nfc):
                bg, bu = gu_banks.next(), gu_banks.next()
                for (bk, w) in ((bg, wg[k]), (bu, wu[k])):
                    for kc in range(8):
                        S.op(pe, lambda e, bk=bk, w=w, kc=kc: e.matmul(
                            banks[bk][:, 0:n], lhsT=w[:, kc, fc * 128:(fc + 1) * 128], rhs=xT[:, kc, c0:c0 + n],
                            start=(kc == 0), stop=(kc == 7)),
                             reads=[r_w[k]] + rx, writes=[r_bank[bk]], signal=(kc == 7))
                sgt, r_sg = sg.next()
                S.op(act, lambda e: e.activation(out=sgt[:, 0:n], in_=banks[bg][:, 0:n], func=AF.Silu),
                     reads=[r_bank[bg]], writes=[r_sg])
                S.op(dve, lambda e: e.tensor_tensor(out=G[gi][:, fc, 0:n], in0=banks[bu][:, 0:n], in1=sgt[:, 0:n],
                                                    op=ALU.mult),
                     reads=[r_bank[bu], r_sg], writes=[r_G[gi]])

        def stage_b(jb):
            u, c0, n = jb["u"], jb["c0"], jb["n"]
            k = u % NW
            nfc = unit_srcs[u][3]
            gi = jb["idx"] % 2
            for t in tiles_of(c0, n):
                r0, r = TOKT[t]
                off = r0 - c0
                yt = ytile_of(t)
                for half in range(2):
                    by = y_banks.next()
                    for fc in range(nfc):
                        S.op(pe, lambda e, fc=fc: e.matmul(
                            banks[by][0:r, :], lhsT=G[gi][:, fc, off:off + r], rhs=wd[k][:, fc, half * 512:(half + 1) * 512],
                            start=(fc == 0), stop=(fc == nfc - 1)),
                             reads=[r_G[gi], r_w[k]], writes=[r_bank[by]], signal=(fc == nfc - 1))
                    ysl = y[0:r, yt, half * 512:(half + 1) * 512]
                    sc = scale_of(unit_srcs[u][4], yt, r)
                    if sc is None:
                        S.op(dve, lambda e: e.tensor_tensor(out=ysl, in0=banks[by][0:r, :], in1=ysl, op=ALU.add),
                             reads=[r_bank[by], r_y[yt]], writes=[r_y[yt]])
                    else:
                        sc_ap, r_sc = sc
                        S.op(dve, lambda e: e.scalar_tensor_tensor(out=ysl, in0=banks[by][0:r, :], scalar=sc_ap, in1=ysl,
                                                                   op0=ALU.mult, op1=ALU.add),
                             reads=[r_bank[by], r_y[yt], r_sc], writes=[r_y[yt]])
                if on_tile_done is not None and u == len(unit_srcs) - 1:
                    on_tile_done(t, yt)

        pipeline(jobs, [stage_a, stage_b])

    for s in range(nseq):
        with ExitStack() as l1:
            oT = sb(l1, "oT", [128, 8, L], BF16)
            r_oT = mkres(17)
            with ExitStack() as sa:
                xT = sb(sa, "xn0T", [128, 8, L], BF16)
                r_xT = mkres(17)
                gb = sb(sa, "gb0", [128, D], F32)
                r_gb = Res()
                load_gb(gb, r_gb, g_a_d)
                hbuf = [sb(sa, "hb%d" % i, [128, D], F32) for i in range(4)]
                r_hb = mkres(4)
                nw = NormWork(sa, "n0", nbuf=4)
                items = []
                for t, (r0, r) in enumerate(TOKT):
                    k3 = t % 4
                    items.append(dict(
                        pre=(lambda t=t, r=r, k3=k3: S.dma(sp, hbuf[k3][0:r, :], h0_src(s, t), writes=[r_hb[k3]])),
                        h=hbuf[k3][0:r, :], r=r, r_h=r_hb[k3],
                        outs=[(gb, r_gb, xT, r0, r_xT[t], 6 + (t % 2))]))
                norm_pipeline(nw, items)

                NB = 2
                wqkv = [sb(sa, "wqkv%d" % i, [128, 3, 8, 128], BF16) for i in range(NB)]
                r_wq = mkres(NB)
                QT = [sb(sa, "QT%d" % i, [128, L], BF16) for i in range(NB)]
                KT = [sb(sa, "KT%d" % i, [128, L], BF16) for i in range(NB)]
                V = [sb(sa, "V%d" % i, [128, 17, 128], BF16) for i in range(NB)]
                r_Q = [mkres(len(COLT)) for _ in range(NB)]
                r_K = [mkres(17) for _ in range(NB)]
                r_V = [mkres(17) for _ in range(NB)]
                Ebuf = Rot([(sb(sa, "E%d" % i, [128, 2, 512], F32), Res()) for i in range(5)])
                Pbuf = Rot([(sb(sa, "P%d" % i, [128, 2, 512], F32), Res()) for i in range(2)])
                SPbuf = Rot([(sb(sa, "SP%d" % i, [128, 2, 512], BF16), Res()) for i in range(2)])
                Abuf = Rot([(sb(sa, "A%d" % i, [128, 2, 512], BF16), Res()) for i in range(2)])
                Cbuf = [(sb(sa, "C%d" % i, [128, 2, 512], BF16), Res()) for i in range(2)]
                r_za, r_eb = Res(), Res()
                r_zas1 = mkres(2)
                NDUMMY_SB = 0
                wqv = wqkv_d[0].rearrange("(kc p) n -> p kc n", p=128)

                def load_pair_w(j):
                    jb = j % NB
                    for i in range(3):
                        S.dma(pool, wqkv[jb][:, i], wqv[:, :, i * D + j * 128:i * D + (j + 1) * 128], writes=[r_wq[jb]])

                load_pair_w(0)
                pbank = Rot([6, 7])
                for j in range(8):
                    jb = j % NB
                    if j + 1 < 8:
                        load_pair_w(j + 1)
                    for ci, (c0, n) in enumerate(COLT):
                        rx = [r_xT[t] for t in tiles_of(c0, n)]
                        for i, dst, rdst, scl in ((0, QT[jb], [r_Q[jb][ci]], HD ** -0.5),
                                                  (1, KT[jb], [r_K[jb][t] for t in tiles_of(c0, n)], 1.0)):
                            bk = pbank.next()
                            for kc in range(8):
                                S.op(pe, lambda e, kc=kc, i=i, bk=bk: e.matmul(
                                    banks[bk][:, 0:n], lhsT=wqkv[jb][:, i, kc, :], rhs=xT[:, kc, c0:c0 + n],
                                    start=(kc == 0), stop=(kc == 7)),
                                     reads=[r_wq[jb]] + rx, writes=[r_bank[bk]], signal=(kc == 7))
                            S.op(dve, lambda e, dst=dst, scl=scl, bk=bk: e.tensor_scalar(
                                out=dst[:, c0:c0 + n], in0=banks[bk][:, 0:n], scalar1=scl, scalar2=None, op0=ALU.mult),
                                 reads=[r_bank[bk]], writes=rdst)
                    for t, (r0, r) in enumerate(TOKT):
                        bk = pbank.next()
                        for kc in range(8):
                            S.op(pe, lambda e, kc=kc, bk=bk: e.matmul(
                                banks[bk][0:r, 0:128], lhsT=xT[:, kc, r0:r0 + r], rhs=wqkv[jb][:, 2, kc, :],
                                start=(kc == 0), stop=(kc == 7)),
                                 reads=[r_wq[jb], r_xT[t]], writes=[r_bank[bk]], signal=(kc == 7))
                        S.op(dve, lambda e, bk=bk: e.tensor_copy(out=V[jb][0:r, t, :], in_=banks[bk][0:r, 0:128]),
                             reads=[r_bank[bk]], writes=[r_V[jb][t]])

                    blocks = []
                    for qi, (qc0, n) in enumerate(COLT):
                        kts = [t for t in range(16, 0, -1) if TOKT[t][0] < qc0 + n] + [0]
                        if qi == 0:
                            kts = [0]
                        for bi, kt in enumerate(kts):
                            kc0, r = TOKT[kt]
                            qlo = max(0, kc0 - qc0)
                            masked = (kc0 + r - 1 >= qc0 + qlo)
                            blocks.append(dict(qi=qi, qc0=qc0, n=n, kt=kt, kc0=kc0, r=r, qlo=qlo, masked=masked,
                                               first=(bi == 0), last=(bi == len(kts) - 1)))
                    HP = (slice(0, 64), slice(64, 128))
                    for bi_, b_ in enumerate(blocks):
                        b_["zs"] = bi_ % 2

                    def st1(b):
                        r, qlo, n, kc0, qc0 = b["r"], b["qlo"], b["n"], b["kc0"], b["qc0"]
                        for hh in range(2):
                            S.op(pe, lambda e: e.matmul(PS[0:r, 2 * b["zs"] + hh, qlo:n], lhsT=KT[jb][HP[hh], kc0:kc0 + r],
                                                        rhs=QT[jb][HP[hh], qc0 + qlo:qc0 + n], start=True, stop=True),
                                 reads=[r_K[jb][b["kt"]], r_Q[jb][b["qi"]]], writes=[r_zas1[b["zs"]]], signal=(hh == 1))

                    def st2a(b):
                        r, qlo, n = b["r"], b["qlo"], b["n"]
                        Et, r_E = Ebuf.next()
                        b["E"], b["r_E"] = Et, r_E
                        zs = b["zs"]
                        S.op(act, lambda e: e.activation(out=Et[0:r, :, qlo:n], in_=PS[0:r, 2 * zs:2 * zs + 2, qlo:n], func=AF.Exp),
                             reads=[r_zas1[zs]], writes=[r_E])

                    def st2b(b):
                        r, qlo, n, kc0, qc0 = b["r"], b["qlo"], b["n"], b["kc0"], b["qc0"]
                        Et, r_E = b["E"], b["r_E"]
                        SPt, r_SP = SPbuf.next()
                        b["SP"], b["r_SP"] = SPt, r_SP
                        S.op(act, lambda e: e.activation(out=SPt[0:r, :, qlo:n], in_=Et[0:r, :, qlo:n], func=AF.Ln, bias=1.0),
                             reads=[r_E], writes=[r_SP])
                        if b["masked"]:
                            S.op(pool, lambda e: e.affine_select(out=SPt[0:r, :, qlo:n], in_=SPt[0:r, :, qlo:n],
                                                                 pattern=[[0, 2], [1, n - qlo]], compare_op=ALU.is_gt,
                                                                 fill=0.0, base=qc0 + qlo - kc0, channel_multiplier=-1),
                                 reads=[r_SP], writes=[r_SP])

                    def st3(b):
                        r, qlo, n, kc0, qc0 = b["r"], b["qlo"], b["n"], b["kc0"], b["qc0"]
                        Ct, r_C = Cbuf[b["qi"] % 2]
                        SPt, r_SP = b["SP"], b["r_SP"]
                        if b["first"] and not b["last"]:
                            S.op(dve, lambda e: e.memset(Ct[:, :, :], 0.0), writes=[r_C])
                        for hh in range(2):
                            S.op(pe, lambda e: e.matmul(PS[0:r, 4 + hh, qlo:n], lhsT=trineg_b[0:r, 0:r],
                                                        rhs=SPt[0:r, hh, qlo:n], start=True, stop=b["first"]),
                                 reads=[r_SP, r_c], writes=[r_eb], signal=(b["first"] and hh == 1))
                            if not b["first"]:
                                S.op(pe, lambda e: e.matmul(PS[0:r, 4 + hh, qlo:n], lhsT=neg1_b[:, 0:r], rhs=Ct[:, hh, qlo:n],
                                                            start=False, stop=True),
                                     reads=[r_C], writes=[r_eb], signal=(hh == 1))
                        if not b["last"]:
                            S.op(dve, lambda e: e.tensor_tensor(out=Ct[:, :, qlo:n], in0=Ct[:, :, qlo:n], in1=SPt[:, :, qlo:n],
                                                                op=ALU.add),
                                 reads=[r_C, r_SP], writes=[r_C])

                    def st4(b):
                        r, qlo, n, kc0, qc0 = b["r"], b["qlo"], b["n"], b["kc0"], b["qc0"]
                        At, r_A = Abuf.next()
                        b["A"], b["r_A"] = At, r_A
                        Pt, r_P = Pbuf.next()
                        Et, r_E = b["E"], b["r_E"]
                        S.op(act, lambda e: e.activation(out=Pt[0:r, :, qlo:n], in_=PS[0:r, 4:6, qlo:n], func=AF.Exp),
                             reads=[r_eb], writes=[r_P])
                        S.op(dve, lambda e: e.tensor_tensor(out=At[0:r, :, qlo:n], in0=Et[0:r, :, qlo:n], in1=Pt[0:r, :, qlo:n],
                                                            op=ALU.mult),
                             reads=[r_E, r_P], writes=[r_A])
                        if b["masked"]:
                            S.op(pool, lambda e: e.affine_select(out=At[0:r, :, qlo:n], in_=At[0:r, :, qlo:n],
                                                                 pattern=[[0, 2], [1, n - qlo]], compare_op=ALU.is_gt,
                                                                 fill=0.0, base=qc0 + qlo - kc0, channel_multiplier=-1),
                                 reads=[r_A], writes=[r_A])

                    def st5(b):
                        bo = 6 + (b["qi"] % 2)
                        r, qlo, n, qc0 = b["r"], b["qlo"], b["n"], b["qc0"]
                        At, r_A = b["A"], b["r_A"]
                        for hh in range(2):
                            S.op(pe, lambda e: e.matmul(PS[HP[hh], bo, qlo:n], lhsT=V[jb][0:r, b["kt"], HP[hh]],
                                                        rhs=At[0:r, hh, qlo:n], start=b["first"], stop=b["last"],
                                                        skip_group_check=True),
                                 reads=[r_V[jb][b["kt"]], r_A], writes=[r_bank[bo]], signal=(hh == 1))
                        if b["last"]:
                            S.op(dve, lambda e: e.tensor_copy(out=oT[:, j, qc0:qc0 + n], in_=PS[:, bo, 0:n]),
                                 reads=[r_bank[bo]], writes=[r_oT[t] for t in tiles_of(qc0, n)])

                    pipeline_rev(blocks, [st1, st2a, st2b, st3, st4, st5])
            S.barrier()
            if stop_after <= 1:
                continue

            with ExitStack() as sbk:
                y = sb(sbk, "y", [128, 17, D], F32)
                r_y = mkres(17)
                with ExitStack() as s2:
                    wo = sb(s2, "wo", [128, 8, D], BF16)
                    r_wo = Res()
                    S.dma(pool, wo[:], woa_d[0].rearrange("(kc p) n -> p kc n", p=128), writes=[r_wo])
                    gb = sb(s2, "gb1", [128, D], F32)
                    r_gb = Res()
                    load_gb(gb, r_gb, g_fd_d)
                    hbuf = [sb(s2, "hb%d" % i, [128, D], F32) for i in range(4)]
                    r_hb = mkres(4)
                    nw = NormWork(s2, "n1", nbuf=4)
                    obank = Rot([0, 1, 2, 3])
                    items = []
                    for t, (r0, r) in enumerate(TOKT):
                        k3 = t % 4

                        def pre(t=t, r0=r0, r=r, k3=k3):
                            S.dma(sp, hbuf[k3][0:r, :], h0_src(s, t), writes=[r_hb[k3]])
                            for half in range(2):
                                bk = obank.next()
                                for jj in range(8):
                                    S.op(pe, lambda e, jj=jj, bk=bk: e.matmul(
                                        banks[bk][0:r, :], lhsT=oT[:, jj, r0:r0 + r],
                                        rhs=wo[:, jj, half * 512:(half + 1) * 512], start=(jj == 0), stop=(jj == 7)),
                                         reads=[r_oT[t], r_wo], writes=[r_bank[bk]], signal=(jj == 7))
                                S.op(dve, lambda e, bk=bk: e.tensor_tensor(
                                    out=y[0:r, t, half * 512:(half + 1) * 512], in0=banks[bk][0:r, :],
                                    in1=hbuf[k3][0:r, half * 512:(half + 1) * 512], op=ALU.add),
                                     reads=[r_bank[bk], r_hb[k3]], writes=[r_y[t]])
                            if debug:
                                S.dma(sp, dbg["h1"][s, r0:r0 + r, :], y[0:r, t, :], reads=[r_y[t]])

                        items.append(dict(pre=pre, h=y[0:r, t, :], r=r, r_h=r_y[t],
                                          outs=[(gb, r_gb, oT, r0, r_oT[t], 6 + (t % 2))]))
                    norm_pipeline(nw, items)
                S.barrier()
                if stop_after >= 3:
                    with ExitStack() as s3:
                        srcs = []
                        for (f0, nfc) in UNITS:
                            srcs.append((wgud_d[0][:, f0 * 128:(f0 + nfc) * 128],
                                         wgud_d[0][:, DFF + f0 * 128:DFF + (f0 + nfc) * 128],
                                         wdd_d[0][f0 * 128:(f0 + nfc) * 128, :], nfc, None))
                        ffn_units(s3, oT, r_oT, COLT, y, r_y, lambda t: t, srcs, lambda key, yt, r: None,
                                  on_tile_done=lambda t, yt: S.dma(sp, h2_d[s, TOKT[t][0]:TOKT[t][0] + TOKT[t][1], :],
                                                                   y[0:TOKT[t][1], t, :], reads=[r_y[t]]))
                S.barrier()
            if stop_after <= 3:
                continue

            with ExitStack() as sc:
                xkT = sb(sc, "xkvT", [128, 8, L], BF16)
                r_xk = mkres(17)
                xqT = sb(sc, "xqT", [128, 8, SEQ], BF16)
                r_xq = mkres(17)
                with ExitStack() as s4a:
                    gbk = sb(s4a, "gbk", [128, D], F32)
                    gbq = sb(s4a, "gbq", [128, D], F32)
                    r_gbk, r_gbq = Res(), Res()
                    load_gb(gbk, r_gbk, g_kv_d)
                    load_gb(gbq, r_gbq, g_b_d)
                    hbuf = [sb(s4a, "hb%d" % i, [128, D], F32) for i in range(4)]
                    r_hb = mkres(4)
                    nw = NormWork(s4a, "n4", nbuf=6)
                    items = []
                    for t, (r0, r) in enumerate(TOKT):
                        k3 = t % 4
                        outs = [(gbk, r_gbk, xkT, r0, r_xk[t], 6)]
                        if t > 0:
                            outs.append((gbq, r_gbq, xqT, r0 - NM, r_xq[t], 7))
                        items.append(dict(
                            pre=(lambda t=t, r0=r0, r=r, k3=k3: S.dma(sp, hbuf[k3][0:r, :], h2_d[s, r0:r0 + r, :],
                                                                        writes=[r_hb[k3]])),
                            h=hbuf[k3][0:r, :], r=r, r_h=r_hb[k3], outs=outs))
                    norm_pipeline(nw, items)
                S.barrier()

                wf = sb(sc, "wf", [128, 8, NH], BF16)
                r_wf = Res()
                S.dma(pool, wf[:], wkvf_d.rearrange("(kc p) n -> p kc n", p=128)[:, :, 2 * D:2 * D + NH], writes=[r_wf])
                spf = sb(sc, "spf", [128, 17, NH], F32)
                negF = sb(sc, "negF", [128, 17, NH], F32)
                Rb = sb(sc, "Rb", [128, 17, NH], F32)
                r_spf, r_negF, r_Rb = mkres(17), Res(), Res()
                fl = [sb(sc, "fl%d" % i, [128, 2, NH], F32) for i in range(2)]
                r_fl = mkres(2)
                S.op(dve, lambda e: e.memset(negF[:], 0.0), writes=[r_negF])
                for t, (r0, r) in enumerate(TOKT):
                    kf = t % 2
                    for kc in range(8):
                        S.op(pe, lambda e, kc=kc: e.matmul(banks[4][0:r, 0:NH], lhsT=xkT[:, kc, r0:r0 + r], rhs=wf[:, kc, :],
                                                           start=(kc == 0), stop=(kc == 7)),
                             reads=[r_xk[t], r_wf], writes=[r_bank[4]], signal=(kc == 7))
                    S.op(dve, lambda e: e.tensor_tensor(out=fl[kf][0:r, 0, :], in0=banks[4][0:r, 0:NH], in1=bfb[0:r, :],
                                                        op=ALU.add),
                         reads=[r_bank[4], r_par], writes=[r_fl[kf]])
                    S.op(act, lambda e: e.activation(out=fl[kf][0:r, 1, :], in_=fl[kf][0:r, 0, :], func=AF.Exp, scale=-1.0),
                         reads=[r_fl[kf]], writes=[r_fl[kf]])
                    S.op(act, lambda e: e.activation(out=spf[0:r, t, :], in_=fl[kf][0:r, 1, :], func=AF.Ln, bias=1.0),
                         reads=[r_fl[kf]], writes=[r_spf[t]])
                    S.op(pe, lambda e: e.matmul(banks[5][:, 0:NH], lhsT=ones_f[0:r, :], rhs=spf[0:r, t, :],
                                                start=True, stop=True),
                         reads=[r_spf[t], r_c], writes=[r_bank[5]])
                    S.op(pe, lambda e: e.matmul(banks[6][0:r, 0:NH], lhsT=trile_f[0:r, 0:r], rhs=spf[0:r, t, :],
                                                start=True, stop=True),
                         reads=[r_spf[t], r_c], writes=[r_bank[6]])
                    if t == 0:
                        S.op(dve, lambda e: e.tensor_copy(out=Rb[:, 0, :], in_=banks[5][:, 0:NH]),
                             reads=[r_bank[5]], writes=[r_Rb])
                        S.op(dve, lambda e: e.tensor_copy(out=negF[0:r, 0, :], in_=banks[6][0:r, 0:NH]),
                             reads=[r_bank[6], r_negF], writes=[r_negF])
                    else:
                        S.op(dve, lambda e: e.tensor_tensor(out=negF[0:r, t, :], in0=banks[6][0:r, 0:NH],
                                                            in1=Rb[0:r, t - 1, :], op=ALU.add),
                             reads=[r_bank[6], r_Rb, r_negF], writes=[r_negF])
                        S.op(dve, lambda e: e.tensor_tensor(out=Rb[:, t, :], in0=banks[5][:, 0:NH], in1=Rb[:, t - 1, :],
                                                            op=ALU.add),
                             reads=[r_bank[5], r_Rb], writes=[r_Rb])

                negFT = sb(sc, "negFT", [NH, L], F32)
                r_nFT = Res()
                for t, (r0, r) in enumerate(TOKT):
                    S.op(pe, lambda e: e.matmul(banks[7][0:NH, 0:r], lhsT=negF[0:r, t, :], rhs=id_f[0:r, 0:r],
                                                start=True, stop=True),
                         reads=[r_negF, r_c], writes=[r_bank[7]])
                    S.op(dve, lambda e: e.tensor_copy(out=negFT[:, r0:r0 + r], in_=banks[7][0:NH, 0:r]),
                         reads=[r_bank[7]], writes=[r_nFT])

                NB = 2
                wkv = [sb(sc, "wkv%d" % i, [128, 3, 8, 128], BF16) for i in range(NB)]
                r_wk = mkres(NB)
                QT = [sb(sc, "QT%d" % i, [128, SEQ], BF16) for i in range(NB)]
                KT = [sb(sc, "KT%d" % i, [128, L], BF16) for i in range(NB)]
                V = [sb(sc, "V%d" % i, [128, 17, 128], BF16) for i in range(NB)]
                r_Q = [mkres(4) for _ in range(NB)]
                r_K = [mkres(17) for _ in range(NB)]
                r_V = [mkres(17) for _ in range(NB)]
                sqb = Rot([(sb(sc, "sq%d" % i, [128, 512], BF16), Res()) for i in range(2)])
                rsb = Rot([(sb(sc, "rs%d" % i, [128, 2, 512], F32), Res()) for i in range(2)])
                ZZbuf = Rot([(sb(sc, "ZZ%d" % i, [128, 2, 512], F32), Res()) for i in range(2)])
                Abuf = Rot([(sb(sc, "A%d" % i, [128, 2, 512], BF16), Res()) for i in range(2)])
                FBbuf = [(sb(sc, "FB%d" % i, [128, 2, 512], F32), Res()) for i in range(2)]
                FKbuf = Rot([(sb(sc, "FK%d" % i, [128, 2, 512], F32), Res()) for i in range(3)])
                rden = Rot([(sb(sc, "rden%d" % i, [128, 512], F32), Res()) for i in range(2)])
                r_zas = mkres(2)
                NDUMMY_FOX = 0
                wkvv = wkvf_d.rearrange("(kc p) n -> p kc n", p=128)
                wqbv = wqb_d[0].rearrange("(kc p) n -> p kc n", p=128)

                def load_pair_w1(j):
                    jb = j % NB
                    S.dma(pool, wkv[jb][:, 0], wqbv[:, :, j * 128:(j + 1) * 128], writes=[r_wk[jb]])
                    S.dma(pool, wkv[jb][:, 1], wkvv[:, :, j * 128:(j + 1) * 128], writes=[r_wk[jb]])
                    S.dma(pool, wkv[jb][:, 2], wkvv[:, :, D + j * 128:D + (j + 1) * 128], writes=[r_wk[jb]])

                load_pair_w1(0)
                pbank = Rot([6, 7])
                sbank = Rot([4, 5])
                QCOL = [(512 * i, 512) for i in range(4)]
                for j in range(8):
                    jb = j % NB
                    if j + 1 < 8:
                        load_pair_w1(j + 1)

                    def proj_norm(i, src, r_src_of, c0, n, dst, rdst, nvec):
                        bk = pbank.next()
                        for kc in range(8):
                            S.op(pe, lambda e, kc=kc: e.matmul(banks[bk][:, 0:n], lhsT=wkv[jb][:, i, kc, :],
                                                               rhs=src[:, kc, c0:c0 + n], start=(kc == 0), stop=(kc == 7)),
                                 reads=[r_wk[jb]] + r_src_of, writes=[r_bank[bk]], signal=(kc == 7))
                        sqt, r_sq = sqb.next()
                        S.op(act, lambda e: e.activation(out=sqt[:, 0:n], in_=banks[bk][:, 0:n], func=AF.Square),
                             reads=[r_bank[bk]], writes=[r_sq])
                        bs = sbank.next()
                        S.op(pe, lambda e: e.matmul(banks[bs][:, 0:n], lhsT=blk_b, rhs=sqt[:, 0:n], start=True, stop=True),
                             reads=[r_sq, r_c], writes=[r_bank[bs]])
                        rst, r_rs = rsb.next()
                        S.op(act, lambda e: e.activation(out=rst[:, 0, 0:n], in_=banks[bs][:, 0:n], func=AF.Ln,
                                                         scale=1.0 / HD, bias=EPS),
                             reads=[r_bank[bs]], writes=[r_rs])
                        S.op(act, lambda e: e.activation(out=rst[:, 1, 0:n], in_=rst[:, 0, 0:n], func=AF.Exp, scale=-0.5),
                             reads=[r_rs], writes=[r_rs])
                        S.op(dve, lambda e: e.scalar_tensor_tensor(out=dst, in0=banks[bk][:, 0:n], scalar=nvec[:, 0:1],
                                                                   in1=rst[:, 1, 0:n], op0=ALU.mult, op1=ALU.mult),
                             reads=[r_bank[bk], r_rs, r_par], writes=rdst)

                    for ci, (c0, n) in enumerate(COLT):
                        tl = tiles_of(c0, n)
                        proj_norm(1, xkT, [r_xk[t] for t in tl], c0, n, KT[jb][:, c0:c0 + n], [r_K[jb][t] for t in tl], knorm2)
                    for ci, (c0, n) in enumerate(QCOL):
                        tl = [1 + 4 * ci + i for i in range(4)]
                        proj_norm(0, xqT, [r_xq[t] for t in tl], c0, n, QT[jb][:, c0:c0 + n], [r_Q[jb][ci]], qnorm2)
                    for t, (r0, r) in enumerate(TOKT):
                        bk = pbank.next()
                        for kc in range(8):
                            S.op(pe, lambda e, kc=kc, bk=bk: e.matmul(
                                banks[bk][0:r, 0:128], lhsT=xkT[:, kc, r0:r0 + r], rhs=wkv[jb][:, 2, kc, :],
                                start=(kc == 0), stop=(kc == 7)),
                                 reads=[r_wk[jb], r_xk[t]], writes=[r_bank[bk]], signal=(kc == 7))
                        S.op(dve, lambda e, bk=bk: e.tensor_copy(out=V[jb][0:r, t, :], in_=banks[bk][0:r, 0:128]),
                             reads=[r_bank[bk]], writes=[r_V[jb][t]])
                    blocks = []
                    for qi, (qc0, n) in enumerate(QCOL):
                        kts = [0] + [t for t in range(1, 17) if TOKT[t][0] - NM < qc0 + n]
                        for bi, kt in enumerate(kts):
                            kc0, r = TOKT[kt]
                            kq = kc0 - NM
                            qlo = max(0, kq - qc0)
                            masked = (kq + r - 1 >= qc0 + qlo)
                            blocks.append(dict(qi=qi, qc0=qc0, n=n, kt=kt, kc0=kc0, r=r, qlo=qlo, masked=masked,
                                               kq=kq, first=(bi == 0), last=(bi == len(kts) - 1)))
                    HP = (slice(0, 64), slice(64, 128))

                    def make_fb(qi):
                        qc0_, n_ = QCOL[qi]
                        FBt, r_FB = FBbuf[qi % 2]
                        for hh in range(2):
                            S.op(pe, lambda e: e.matmul(banks[6][:, :], lhsT=SelAll[:, 2 * j + hh, :],
                                                        rhs=negFT[:, NM + qc0_:NM + qc0_ + n_], start=True, stop=True),
                                 reads=[r_nFT, r_par], writes=[r_bank[6]])
                            S.op(dve, lambda e: e.tensor_scalar(out=FBt[:, hh, :], in0=banks[6][:, :], scalar1=-1.0,
                                                                scalar2=None, op0=ALU.mult),
                                 reads=[r_bank[6]], writes=[r_FB])

                    for bi_, b_ in enumerate(blocks):
                        b_["zs"] = bi_ % 2

                    def f1(b):
                        r, qlo, n, kc0, qc0 = b["r"], b["qlo"], b["n"], b["kc0"], b["qc0"]
                        zs = b["zs"]
                        for hh in range(2):
                            S.op(pe, lambda e: e.matmul(PS[0:r, 2 * zs + hh, qlo:n], lhsT=KT[jb][HP[hh], kc0:kc0 + r],
                                                        rhs=QT[jb][HP[hh], qc0 + qlo:qc0 + n], start=True, stop=True),
                                 reads=[r_K[jb][b["kt"]], r_Q[jb][b["qi"]]], writes=[r_zas[zs]], signal=(hh == 1))

                    def f0(b):
                        r, qlo, n = b["r"], b["qlo"], b["n"]
                        if b["first"]:
                            if b["qi"] == 0:
                                make_fb(0)
                            if b["qi"] + 1 < len(QCOL):
                                make_fb(b["qi"] + 1)
                        FBt, r_FB = FBbuf[b["qi"] % 2]
                        FKt, r_FK = FKbuf.next()
                        b["FK"], b["r_FK"] = FKt, r_FK
                        for hh in range(2):
                            h = 2 * j + hh
                            S.op(pool, lambda e: e.tensor_scalar(out=FKt[0:r, hh, qlo:n], in0=FBt[0:r, hh, qlo:n],
                                                                 scalar1=negF[0:r, b["kt"], h:h + 1], scalar2=1.0,
                                                                 op0=ALU.add, op1=ALU.mult),
                                 reads=[r_FB, r_negF], writes=[r_FK])

                    def f1b(b):
                        r, qlo, n = b["r"], b["qlo"], b["n"]
                        zs = b["zs"]
                        FKt, r_FK = b["FK"], b["r_FK"]
                        ZZt, r_ZZ = ZZbuf.next()
                        b["ZZ"], b["r_ZZ"] = ZZt, r_ZZ
                        S.op(dve, lambda e: e.tensor_tensor(out=ZZt[0:r, :, qlo:n], in0=PS[0:r, 2 * zs:2 * zs + 2, qlo:n],
                                                            in1=FKt[0:r, :, qlo:n], op=ALU.add),
                             reads=[r_zas[zs], r_FK], writes=[r_ZZ])

                    def f2(b):
                        r, qlo, n, qc0, kq = b["r"], b["qlo"], b["n"], b["qc0"], b["kq"]
                        ZZt, r_ZZ = b["ZZ"], b["r_ZZ"]
                        At, r_A = Abuf.next()
                        b["A"], b["r_A"] = At, r_A
                        S.op(act, lambda e: e.activation(out=At[0:r, :, qlo:n], in_=ZZt[0:r, :, qlo:n], func=AF.Exp),
                             reads=[r_ZZ], writes=[r_A])
                        if b["masked"]:
                            S.op(pool, lambda e: e.affine_select(out=At[0:r, :, qlo:qlo + 128], in_=At[0:r, :, qlo:qlo + 128],
                                                                 pattern=[[0, 2], [1, 128]], compare_op=ALU.is_ge, fill=0.0,
                                                                 base=qc0 + qlo - kq, channel_multiplier=-1),
                                 reads=[r_A], writes=[r_A])

                    def f3(b):
                        bn, bd = 4, 5
                        r, qlo, n, qc0 = b["r"], b["qlo"], b["n"], b["qc0"]
                        At, r_A = b["A"], b["r_A"]
                        for _ in range(NDUMMY_FOX):
                            S.op(pe, lambda e: e.matmul(banks[7][:, :], lhsT=cb[:, 0:128], rhs=KT[jb][:, 0:512],
                                                        start=True, stop=True),
                                 reads=[r_c] + [r_K[jb][t] for t in range(5)], writes=[r_bank[7]], signal=False)
                        for hh in range(2):
                            S.op(pe, lambda e: e.matmul(PS[HP[hh], bn, qlo:n], lhsT=V[jb][0:r, b["kt"], HP[hh]],
                                                        rhs=At[0:r, hh, qlo:n], start=b["first"], stop=b["last"],
                                                        skip_group_check=True),
                                 reads=[r_V[jb][b["kt"]], r_A], writes=[r_bank[bn]], signal=(hh == 1))
                        for hh in range(2):
                            S.op(pe, lambda e: e.matmul(PS[HP[hh], bd, qlo:n], lhsT=ones_b[0:r, 0:64],
                                                        rhs=At[0:r, hh, qlo:n], start=b["first"], stop=b["last"],
                                                        skip_group_check=True),
                                 reads=[r_A, r_c], writes=[r_bank[bd]], signal=(hh == 1))
                        if b["last"]:
                            rd, r_rd = rden.next()
                            S.op(dve, lambda e: e.reciprocal(out=rd[:, 0:n], in_=PS[:, bd, 0:n]),
                                 reads=[r_bank[bd]], writes=[r_rd])
                            S.op(dve, lambda e: e.tensor_tensor(out=oT[:, j, NM + qc0:NM + qc0 + n], in0=PS[:, bn, 0:n],
                                                                in1=rd[:, 0:n], op=ALU.mult),
                                 reads=[r_bank[bn], r_rd], writes=[r_oT[t] for t in tiles_of(NM + qc0, n)])

                    pipeline_rev(blocks, [f0, f1, f1b, f2, f3])
            S.barrier()
            if stop_after <= 4:
                continue

            with ExitStack() as sd:
                y = sb(sd, "y2", [128, 16, D], F32)
                r_y = mkres(16)
                comb = sb(sd, "comb", [128, 16, NE], F32)
                r_comb = mkres(16)
                with ExitStack() as s5:
                    wo = sb(s5, "wob", [128, 8, D], BF16)
                    r_wo = Res()
                    S.dma(pool, wo[:], wob_d[0].rearrange("(kc p) n -> p kc n", p=128), writes=[r_wo])
                    gb = sb(s5, "gbm", [128, D], F32)
                    r_gb = Res()
                    load_gb(gb, r_gb, g_fm_d)
                    hbuf = [sb(s5, "hb%d" % i, [128, D], F32) for i in range(4)]
                    r_hb = mkres(4)
                    nw = NormWork(s5, "n5", nbuf=3, want32=True)
                    xhl = [(sb(s5, "xh%d" % i, [128, D], BF16), sb(s5, "xl%d" % i, [128, D], BF16)) for i in range(2)]
                    r_xh, r_xl = mkres(2), mkres(2)
                    xlT = [sb(s5, "xlT%d" % i, [128, 8, 128], BF16) for i in range(2)]
                    r_xlT = mkres(2)
                    rt = [sb(s5, "rt%d" % i, [128, 5, NE], F32) for i in range(2)]
                    r_rt = mkres(2)
                    obank = Rot([0, 1, 2, 3])
                    items = [dict(t=t, r0=TOKT[t][0], yt=t - 1, k3=t % 4, k2=(t - 1) % 2) for t in range(1, 17)]

                    def p5_0(it):
                        t, r0, yt, k3 = it["t"], it["r0"], it["yt"], it["k3"]
                        S.dma(sp, hbuf[k3][:, :], h2_d[s, r0:r0 + 128, :], writes=[r_hb[k3]])
                        for half in range(2):
                            bk = obank.next()
                            for jj in range(8):
                                S.op(pe, lambda e, jj=jj, bk=bk: e.matmul(
                                    banks[bk][:, :], lhsT=oT[:, jj, r0:r0 + 128], rhs=wo[:, jj, half * 512:(half + 1) * 512],
                                    start=(jj == 0), stop=(jj == 7)),
                                     reads=[r_oT[t], r_wo], writes=[r_bank[bk]], signal=(jj == 7))
                            S.op(dve, lambda e, bk=bk: e.tensor_tensor(
                                out=y[:, yt, half * 512:(half + 1) * 512], in0=banks[bk][:, :],
                                in1=hbuf[k3][:, half * 512:(half + 1) * 512], op=ALU.add),
                                 reads=[r_bank[bk], r_hb[k3]], writes=[r_y[yt]])
                        if debug:
                            S.dma(sp, dbg["h3"][s, yt * 128:(yt + 1) * 128, :], y[:, yt, :], reads=[r_y[yt]])

                    def p5_1(it):
                        yt = it["yt"]
                        it["k"], it["rstd"], it["r_rs"] = norm_stats(nw, y[:, yt, :], 128, r_y[yt])

                    def p5_2(it):
                        yt, k2, k = it["yt"], it["k2"], it["k"]
                        rx = nw.res[k][2]
                        xs = nw.xs[k]
                        S.op(dve, lambda e: e.scalar_tensor_tensor(out=xs[:, :], in0=y[:, yt, :], scalar=it["rstd"], in1=gb[:, :],
                                                                   op0=ALU.mult, op1=ALU.mult),
                             reads=[r_y[yt], it["r_rs"], r_gb], writes=[rx])
                        xh, xl = xhl[k2]
                        S.op(act, lambda e: e.activation(out=xh[:, :], in_=xs[:, :], func=AF.Copy),
                             reads=[rx], writes=[r_xh[k2]])
                        S.op(dve, lambda e: e.tensor_tensor(out=xl[:, :], in0=xs[:, :], in1=xh[:, :], op=ALU.subtract),
                             reads=[rx, r_xh[k2]], writes=[r_xl[k2]])
                        for (srcx, r_srcx, bk) in ((xh, r_xh[k2], 4), (xl, r_xl[k2], 5)):
                            psT = banks[bk][:, :].bitcast(BF16).rearrange("p (c n) -> p c n", c=8)
                            for c in range(8):
                                S.op(pe, lambda e, c=c: e.transpose(out=psT[:, c, :], in_=srcx[:, c * 128:(c + 1) * 128],
                                                                    identity=id_b),
                                     reads=[r_srcx, r_c], writes=[r_bank[bk]], signal=(c == 7))

                    def p5_3(it):
                        t, r0, k2 = it["t"], it["r0"], it["k2"]
                        psT = banks[4][:, :].bitcast(BF16).rearrange("p (c n) -> p c n", c=8)
                        S.op(act, lambda e: e.activation(out=oT[:, :, r0:r0 + 128], in_=psT, func=AF.Copy),
                             reads=[r_bank[4]], writes=[r_oT[t]])
                        psT = banks[5][:, :].bitcast(BF16).rearrange("p (c n) -> p c n", c=8)
                        S.op(dve, lambda e: e.tensor_copy(out=xlT[k2][:, :, :], in_=psT),
                             reads=[r_bank[5]], writes=[r_xlT[k2]])
                        terms = [(oT, r_oT[t], r0, 0), (xlT[k2], r_xlT[k2], 0, 0), (oT, r_oT[t], r0, 1)]
                        for ti, (lt, r_lt, c0_, wi) in enumerate(terms):
                            for kc in range(8):
                                S.op(pe, lambda e, kc=kc: e.matmul(banks[6][:, 0:NE], lhsT=lt[:, kc, c0_:c0_ + 128],
                                                                   rhs=wrs[:, wi, kc, :],
                                                                   start=(ti == 0 and kc == 0), stop=(ti == 2 and kc == 7)),
                                     reads=[r_lt, r_par], writes=[r_bank[6]], signal=(ti == 2 and kc == 7))

                    def p5_4(it):
                        yt, k2 = it["yt"], it["k2"]
                        R_, rr_ = rt[k2], r_rt[k2]
                        S.op(dve, lambda e: e.tensor_copy(out=R_[:, 0, :], in_=banks[6][:, 0:NE]),
                             reads=[r_bank[6]], writes=[rr_])
                        S.op(dve, lambda e: e.max(out=R_[:, 1, :], in_=R_[:, 0, :]), reads=[rr_], writes=[rr_])
                        S.op(dve, lambda e: e.tensor_scalar(out=R_[:, 2, 0:1], in0=R_[:, 1, 0:1], scalar1=-1.0, scalar2=None,
                                                            op0=ALU.mult), reads=[rr_], writes=[rr_])
                        S.op(act, lambda e: e.activation(out=R_[:, 3, :], in_=R_[:, 0, :], func=AF.Exp, bias=R_[:, 2, 0:1]),
                             reads=[rr_], writes=[rr_])
                        S.op(dve, lambda e: e.scalar_tensor_tensor(out=R_[:, 4, :], in0=R_[:, 0, :], scalar=R_[:, 1, 1:2],
                                                                   in1=R_[:, 3, :], op0=ALU.is_ge, op1=ALU.mult,
                                                                   accum_out=R_[:, 2, 1:2]),
                             reads=[rr_], writes=[rr_])
                        S.op(dve, lambda e: e.reciprocal(out=R_[:, 2, 2:3], in_=R_[:, 2, 1:2]), reads=[rr_], writes=[rr_])
                        S.op(dve, lambda e: e.tensor_scalar(out=comb[:, yt, :], in0=R_[:, 4, :], scalar1=R_[:, 2, 2:3],
                                                            scalar2=None, op0=ALU.mult),
                             reads=[rr_], writes=[r_comb[yt]])
                        if debug:
                            S.dma(sp, dbg["comb"][s, yt * 128:(yt + 1) * 128, :], comb[:, yt, :], reads=[r_comb[yt]])

                    pipeline_rev(items, [p5_0, p5_1, p5_2, p5_3, p5_4])
                S.barrier()
                if stop_after >= 6:
                    with ExitStack() as s6:
                        srcs = []
                        for ex in range(NE):
                            for (f0, nfc) in UNITS:
                                srcs.append((wgum_d[0, ex][:, f0 * 128:(f0 + nfc) * 128],
                                             wgum_d[0, ex][:, DFF + f0 * 128:DFF + (f0 + nfc) * 128],
                                             wdm_d[0, ex][f0 * 128:(f0 + nfc) * 128, :], nfc, ex))
                        ffn_units(s6, oT, r_oT, COLT[1:], y, r_y, lambda t: t - 1, srcs,
                                  lambda key, yt, r: (comb[0:r, yt, key:key + 1], r_comb[yt]),
                                  on_tile_done=lambda t, yt: S.dma(sp, out_d[s, yt * 128:(yt + 1) * 128, :], y[:, yt, :],
                                                                   reads=[r_y[yt]]))
                else:
                    for yt in range(16):
                        S.dma(sp, out_d[s, yt * 128:(yt + 1) * 128, :], y[:, yt, :], reads=[r_y[yt]])
                S.barrier()
    S.barrier()


_PROG = {}


def _in_map(inputs, b0, nseq):
    f = lambda a: np.ascontiguousarray(np.asarray(a, dtype=np.float32))
    return {
        "x": f(inputs["x"][b0:b0 + nseq]),
        "meta_tokens": f(inputs["meta_tokens"]),
        "norm_attn_a": f(inputs["norm_attn_a"]),
        "w_qkv_a": f(inputs["w_qkv_a"]),
        "w_o_a": f(inputs["w_o_a"]),
        "norm_kv": f(inputs["norm_kv"]).reshape(1, D),
        "w_kvf": f(inputs["w_kvf"]),
        "b_f": f(inputs["b_f"]).reshape(1, NH),
        "k_norm": f(inputs["k_norm"]).reshape(HD, 1),
        "norm_attn_b": f(inputs["norm_attn_b"]),
        "w_q_b": f(inputs["w_q_b"]),
        "q_norm_b": f(inputs["q_norm_b"]).reshape(HD, 1),
        "w_o_b": f(inputs["w_o_b"]),
        "norm_ffn_dense": f(inputs["norm_ffn_dense"]),
        "w_gu_dense": f(inputs["w_gu_dense"]),
        "w_down_dense": f(inputs["w_down_dense"]),
        "norm_ffn_moe": f(inputs["norm_ffn_moe"]),
        "w_router": f(inputs["w_router"]),
        "w_gu_moe": f(inputs["w_gu_moe"]),
        "w_down_moe": f(inputs["w_down_moe"]),
        "consts": make_consts(),
    }


def kernel(**inputs):
    nseq = 2
    nc = build_program(nseq=nseq)
    in_maps = [_in_map(inputs, nseq * c, nseq) for c in range(NCORES)]
    res = run_bass_kernel_spmd(nc, in_maps, core_ids=list(range(NCORES)))
    out = np.concatenate([np.asarray(r["out"]) for r in res.results], axis=0)
    return out.astype(np.float32)
```
